# Optimizing a Trainium2 kernel written in Bass

```python
import math
import jax
import jax.numpy as jnp
from jax import lax
import numpy as np

D_MODEL = 1024
BATCH = 8
SEQ = 4096
DEPTH = 1

D_MIX = D_MODEL
SB_HEADS = 8
SB_HEAD_DIM = 64
SB_WIDTH = SB_HEADS * SB_HEAD_DIM
POOL_WINDOWS = (2, 4, 8, 16)
POOL_GROUPS = len(POOL_WINDOWS)
POOL_WIDTH = D_MIX - SB_WIDTH
POOL_GROUP_DIM = POOL_WIDTH // POOL_GROUPS
IN_PROJ_COLS = 3 * SB_WIDTH + POOL_WIDTH
Q_BLOCK = 128
N_MEM = 256
MEM_HEADS = 4
MEM_HEAD_DIM = D_MODEL // MEM_HEADS
N_EXPERTS = 32
TOP_K = 4
D_FF = D_MODEL
SWIGLU_LIMIT = 7.0
SWIGLU_ALPHA = 1.702
MOE_BLOCK = 128
RMS_EPS = 1e-5

kernel_name = "hybrid_stickbreak_pool_memxattn_moe"


def rmsnorm(x, g):
    xf = x.astype(jnp.float32)
    y = xf * lax.rsqrt(jnp.mean(xf * xf, axis=-1, keepdims=True) + RMS_EPS)
    return (y * g.astype(jnp.float32)).astype(x.dtype)


def stick_breaking_attention(q, k, v):
    seq = q.shape[2]
    scale = 1.0 / math.sqrt(q.shape[-1])
    outs = []
    for i in range(seq // Q_BLOCK):
        t0 = i * Q_BLOCK
        kv_len = t0 + Q_BLOCK
        qb = q[:, :, t0:kv_len]
        kb = k[:, :, :kv_len]
        vb = v[:, :, :kv_len]
        z = jnp.einsum('bhqd,bhkd->bhqk', qb, kb).astype(jnp.float32) * scale
        t_pos = t0 + jnp.arange(Q_BLOCK)
        s_pos = jnp.arange(kv_len)
        mask = s_pos[None, :] < t_pos[:, None]
        log_stay = jnp.where(mask, jax.nn.log_sigmoid(-z), 0.0)
        rev = lax.cumsum(log_stay, axis=3, reverse=True)
        after = jnp.concatenate([rev[..., 1:], jnp.zeros_like(rev[..., :1])], axis=-1)
        w = jnp.where(mask, jnp.exp(jax.nn.log_sigmoid(z) + after), 0.0)
        outs.append(jnp.einsum('bhqk,bhkd->bhqd', w.astype(vb.dtype), vb))
    return jnp.concatenate(outs, axis=2)


def multiscale_pool(u):
    seq = u.shape[1]
    uf = u.astype(jnp.float32)
    csum = jnp.cumsum(uf, axis=1)
    outs = []
    for g, win in enumerate(POOL_WINDOWS):
        cg = csum[:, :, g]
        shifted = jnp.pad(cg, ((0, 0), (win, 0), (0, 0)))[:, :seq]
        count = jnp.minimum(jnp.arange(seq) + 1, win).astype(jnp.float32)
        outs.append((cg - shifted) / count[None, :, None] - uf[:, :, g])
    return jnp.stack(outs, axis=2).astype(u.dtype)


def memory_cross_attention(h, m, w_q, w_kv, w_o):
    b, s, d = h.shape
    q = (h @ w_q).reshape(b, s, MEM_HEADS, MEM_HEAD_DIM)
    kv = m @ w_kv
    k = kv[..., :d].reshape(b, -1, MEM_HEADS, MEM_HEAD_DIM)
    v = kv[..., d:].reshape(b, -1, MEM_HEADS, MEM_HEAD_DIM)
    scores = jnp.einsum('bshd,bmhd->bhsm', q, k).astype(jnp.float32) / math.sqrt(MEM_HEAD_DIM)
    p = jax.nn.softmax(scores, axis=-1).astype(v.dtype)
    o = jnp.einsum('bhsm,bmhd->bshd', p, v).reshape(b, s, d)
    return o @ w_o


def moe_ffn(h, w_router, b_router, w1, b1, w2, b2):
    b, s, d = h.shape
    n_tok = b * s
    hf = h.reshape(n_tok, d)
    logits = (hf @ w_router + b_router).astype(jnp.float32)
    top_val, top_idx = lax.top_k(logits, TOP_K)
    gates = jax.nn.softmax(top_val, axis=-1)
    nk = n_tok * TOP_K
    flat_e = top_idx.reshape(nk)
    flat_tok = (jnp.arange(nk, dtype=jnp.int32) // TOP_K)
    flat_g = gates.reshape(nk)
    order = jnp.argsort(flat_e)
    sorted_e = flat_e[order]
    counts = jnp.bincount(flat_e, length=N_EXPERTS)
    padded = (counts + MOE_BLOCK - 1) // MOE_BLOCK * MOE_BLOCK
    start = jnp.cumsum(counts) - counts
    pend = jnp.cumsum(padded)
    pstart = pend - padded
    dest = pstart[sorted_e] + jnp.arange(nk) - start[sorted_e]
    n_rows = (nk + MOE_BLOCK - 1) // MOE_BLOCK * MOE_BLOCK + N_EXPERTS * MOE_BLOCK
    n_blk = n_rows // MOE_BLOCK
    row_tok = jnp.zeros((n_rows,), jnp.int32).at[dest].set(flat_tok[order])
    row_g = jnp.zeros((n_rows,), h.dtype).at[dest].set(flat_g[order].astype(h.dtype))
    blk_e = jnp.minimum(jnp.searchsorted(pend, jnp.arange(n_blk) * MOE_BLOCK, side='right'),
                        N_EXPERTS - 1)

    def expert_block(args):
        e, tok, g = args
        xb = hf[tok]
        gu = xb @ w1[e] + b1[e]
        gate = jnp.minimum(gu[:, :D_FF], SWIGLU_LIMIT)
        up = jnp.clip(gu[:, D_FF:], -SWIGLU_LIMIT, SWIGLU_LIMIT)
        act = (up + 1.0) * gate * jax.nn.sigmoid(SWIGLU_ALPHA * gate)
        return (act @ w2[e] + b2[e]) * g[:, None]

    y = lax.map(expert_block, (blk_e, row_tok.reshape(n_blk, MOE_BLOCK),
                               row_g.reshape(n_blk, MOE_BLOCK)))
    out = jnp.zeros((n_tok, d), h.dtype).at[row_tok].add(y.reshape(n_rows, d))
    return out.reshape(b, s, d)


def setup_inputs(seed: int = 0) -> dict:
    key = jax.random.key(seed)
    ks = jax.random.split(key, 24)
    f32 = jnp.float32
    nrm = lambda k, shape, fan_in: jax.random.normal(k, shape, f32) * (fan_in ** -0.5)
    gain = lambda k, shape: 1.0 + 0.05 * jax.random.normal(k, shape, f32)
    L = DEPTH
    return {
        "x": jax.random.normal(ks[0], (BATCH, SEQ, D_MODEL), f32),
        "mem": jax.random.normal(ks[1], (BATCH, N_MEM, D_MODEL), f32),
        "g_mix": gain(ks[2], (L, D_MODEL)),
        "w_in": nrm(ks[3], (L, D_MODEL, IN_PROJ_COLS), D_MODEL),
        "g_sb_out": gain(ks[4], (L, SB_WIDTH)),
        "g_pool_out": gain(ks[5], (L, POOL_WIDTH)),
        "w_pool": nrm(ks[6], (L, POOL_GROUPS, POOL_GROUP_DIM, POOL_GROUP_DIM), POOL_GROUP_DIM),
        "pool_scale": 1.0 + 0.1 * jax.random.normal(ks[7], (L, POOL_WIDTH), f32),
        "w_out": nrm(ks[8], (L, D_MIX, D_MODEL), D_MIX),
        "g_mem_q": gain(ks[9], (L, D_MODEL)),
        "g_mem_kv": gain(ks[10], (L, D_MODEL)),
        "w_mem_q": nrm(ks[11], (L, D_MODEL, D_MODEL), D_MODEL),
        "w_mem_kv": nrm(ks[12], (L, D_MODEL, 2 * D_MODEL), D_MODEL),
        "w_mem_o": nrm(ks[13], (L, D_MODEL, D_MODEL), D_MODEL),
        "g_ffn": gain(ks[14], (L, D_MODEL)),
        "w_router": nrm(ks[15], (L, D_MODEL, N_EXPERTS), D_MODEL),
        "b_router": 0.01 * jax.random.normal(ks[16], (L, N_EXPERTS), f32),
        "w_expert_in": nrm(ks[17], (L, N_EXPERTS, D_MODEL, 2 * D_FF), D_MODEL),
        "b_expert_in": 0.01 * jax.random.normal(ks[18], (L, N_EXPERTS, 2 * D_FF), f32),
        "w_expert_out": nrm(ks[19], (L, N_EXPERTS, D_FF, D_MODEL), D_FF),
        "b_expert_out": 0.01 * jax.random.normal(ks[20], (L, N_EXPERTS, D_MODEL), f32),
        "g_final": gain(ks[21], (D_MODEL,)),
    }


def reference(x, mem, g_mix, w_in, g_sb_out, g_pool_out, w_pool, pool_scale, w_out,
              g_mem_q, g_mem_kv, w_mem_q, w_mem_kv, w_mem_o, g_ffn, w_router, b_router,
              w_expert_in, b_expert_in, w_expert_out, b_expert_out, g_final):
    b, s, d = x.shape
    for l in range(DEPTH):
        h = rmsnorm(x, g_mix[l])
        proj = h @ w_in[l]
        q = proj[..., :SB_WIDTH].reshape(b, s, SB_HEADS, SB_HEAD_DIM).transpose(0, 2, 1, 3)
        k = proj[..., SB_WIDTH:2 * SB_WIDTH].reshape(b, s, SB_HEADS, SB_HEAD_DIM).transpose(0, 2, 1, 3)
        v = proj[..., 2 * SB_WIDTH:3 * SB_WIDTH].reshape(b, s, SB_HEADS, SB_HEAD_DIM).transpose(0, 2, 1, 3)
        u = proj[..., 3 * SB_WIDTH:].reshape(b, s, POOL_GROUPS, POOL_GROUP_DIM)
        sb = stick_breaking_attention(q, k, v).transpose(0, 2, 1, 3).reshape(b, s, SB_WIDTH)
        pooled = multiscale_pool(u)
        pooled = jnp.einsum('bsgc,gce->bsge', pooled, w_pool[l]).reshape(b, s, POOL_WIDTH) * pool_scale[l]
        mixed = jnp.concatenate([rmsnorm(sb, g_sb_out[l]), rmsnorm(pooled, g_pool_out[l])], axis=-1)
        x = x + mixed @ w_out[l]
        x = x + memory_cross_attention(rmsnorm(x, g_mem_q[l]), rmsnorm(mem, g_mem_kv[l]),
                                       w_mem_q[l], w_mem_kv[l], w_mem_o[l])
        x = x + moe_ffn(rmsnorm(x, g_ffn[l]), w_router[l], b_router[l], w_expert_in[l],
                        b_expert_in[l], w_expert_out[l], b_expert_out[l])
    return rmsnorm(x, g_final)
```

```python
import contextlib
import numpy as np
import concourse.bass as bass
import concourse.mybir as mybir
from concourse.bass_utils import run_bass_kernel_spmd

F32 = mybir.dt.float32
BF16 = mybir.dt.bfloat16
I32 = mybir.dt.int32
AF = mybir.ActivationFunctionType
ALU = mybir.AluOpType

N_DMA_SEMS = 32
D = 1024
NE = 32
NMEM = 256
EPS = 1e-5


class Tok:
    __slots__ = ("last_w", "readers")

    def __init__(self):
        self.last_w = None
        self.readers = []


class Op:
    __slots__ = ("eng", "fn", "deps", "signaled", "sem", "val", "is_dma")

    def __init__(self, eng, fn, is_dma):
        self.eng = eng
        self.fn = fn
        self.deps = []
        self.signaled = False
        self.sem = None
        self.val = 0
        self.is_dma = is_dma


class Sched:
    ENGS = ("pe", "act", "dve", "pool", "sp")

    def __init__(self, G):
        self.G = G
        self.ops = {e: [] for e in self.ENGS}
        self.dma_rr = {"sp": 0, "pool": 0, "act": 0}
        self.dma_last = [None] * N_DMA_SEMS
        self.dma_cnt = G["dma_cnt"]

    def _add(self, eng, fn, reads, writes, is_dma):
        op = Op(eng, fn, is_dma)
        deps = []
        for t in reads:
            if t.last_w is not None:
                deps.append(t.last_w)
        for t in writes:
            if t.last_w is not None:
                deps.append(t.last_w)
            deps.extend(t.readers)
        if is_dma:
            half = N_DMA_SEMS // 2
            j = self.dma_rr[eng] + (half if eng == "pool" else 0)
            self.dma_rr[eng] = (self.dma_rr[eng] + 1) % half
            if self.dma_last[j] is not None:
                deps.append(self.dma_last[j])
            self.dma_last[j] = op
            self.dma_cnt[j] += 16
            op.sem = ("dma", j)
            op.val = self.dma_cnt[j]
            op.signaled = True
        seen = set()
        for d in deps:
            if id(d) in seen:
                continue
            seen.add(id(d))
            if (not d.is_dma) and (not is_dma) and d.eng == eng and eng == "pe":
                continue
            op.deps.append(d)
            if not d.is_dma:
                d.signaled = True
        for t in reads:
            t.readers.append(op)
        for t in writes:
            t.last_w = op
            t.readers = []
        self.ops[eng].append(op)
        return op

    def op(self, eng, fn, reads=(), writes=()):
        return self._add(eng, fn, reads, writes, False)

    def dma(self, eng, fn, reads=(), writes=()):
        return self._add(eng, fn, reads, writes, True)

    def emit(self, nc, tag):
        for e in self.ENGS:
            for op in reversed(self.ops[e]):
                if not op.is_dma:
                    op.signaled = True
                    break
        for e in self.ENGS:
            c = self.G["eng_cnt"][e]
            for op in self.ops[e]:
                if op.is_dma:
                    continue
                if op.signaled:
                    c += 1
                    op.sem = ("eng", e)
                    op.val = c
            self.G["eng_cnt"][e] = c
        with contextlib.ExitStack() as st:
            sems = self.G["sems"]
            block = st.enter_context(nc.Block())
            lasts = []
            for j in range(N_DMA_SEMS):
                if self.dma_last[j] is not None:
                    lasts.append(self.dma_last[j])
            for e in self.ENGS:
                for op in reversed(self.ops[e]):
                    if not op.is_dma:
                        lasts.append(op)
                        break
            for e in self.ENGS:
                fin = Op(e, None, False)
                fin.deps = list(lasts)
                self.ops[e].append(fin)

            def run(engname, eng):
                waited = {}
                for op in self.ops[engname]:
                    need = {}
                    for d in op.deps:
                        if need.get(d.sem, 0) < d.val:
                            need[d.sem] = d.val
                    for s, v in need.items():
                        if waited.get(s, 0) < v:
                            eng.wait_ge(sems[s], v)
                            waited[s] = v
                    if op.fn is None:
                        continue
                    ins = op.fn(eng)
                    if op.is_dma:
                        ins.then_inc(sems[op.sem], 16)
                    elif op.signaled:
                        ins.then_inc(sems[op.sem], 1)

            block.tensor(lambda eng: run("pe", eng))
            block.scalar(lambda eng: run("act", eng))
            block.vector(lambda eng: run("dve", eng))
            block.gpsimd(lambda eng: run("pool", eng))
            block.sync(lambda eng: run("sp", eng))


class _Any:
    def __getattr__(self, k):
        return self

    def __call__(self, *a, **k):
        return self

    def __getitem__(self, k):
        return self

    def __iter__(self):
        return iter((self, self))


class Skip:
    def __enter__(self):
        return _Any()

    def __exit__(self, *a):
        return False


def run_interleaved(gens, width):
    active = []
    it = iter(gens)
    done = False
    while True:
        while not done and len(active) < width:
            g = next(it, None)
            if g is None:
                done = True
                break
            active.append(g)
        if not active:
            break
        for g in list(active):
            try:
                next(g)
            except StopIteration:
                active.remove(g)


class Phase:
    def __init__(self, nc, tag, G):
        self.nc = nc
        self.tag = tag
        self.S = Sched(G)
        self.st = contextlib.ExitStack()
        self.n = 0

    def __enter__(self):
        self.st.__enter__()
        return self

    def __exit__(self, *a):
        if a[0] is None:
            self.S.emit(self.nc, self.tag)
        return self.st.__exit__(*a)

    def sb(self, shape, dt, name=None, ntok=None):
        self.n += 1
        t = self.st.enter_context(self.nc.sbuf_tensor("%s_%s%d" % (self.tag, name or "t", self.n), list(shape), dt))
        if ntok is not None:
            return t, [Tok() for _ in range(ntok)]
        return t, Tok()

    def ps(self, shape, dt, name=None):
        self.n += 1
        esz = 2 if dt == BF16 else 4
        free = 1
        for d_ in shape[1:]:
            free *= d_
        nbytes = free * esz
        assert nbytes % 2048 == 0 or len(shape) == 2, shape
        shape = list(shape)
        if nbytes % 2048 != 0:
            shape[1] = ((nbytes + 2047) // 2048) * 2048 // esz
        shape[0] = 128
        t = self.st.enter_context(self.nc.psum_tensor("%s_%s%d" % (self.tag, name or "p", self.n), shape, dt))
        return t, Tok()

    def dma(self, q, out, in_, reads=(), writes=()):
        return self.S.dma(q, lambda e: e.dma_start(out=out, in_=in_), reads, writes)

    def mm(self, out, lhsT, rhs, start, stop, reads=(), writes=()):
        return self.S.op("pe", lambda e: e.matmul(out, lhsT, rhs, start=start, stop=stop), reads, writes)

    def tr(self, out, in_, ident, reads=(), writes=()):
        return self.S.op("pe", lambda e: e.transpose(out, in_, ident), reads, writes)

    def act(self, out, in_, func, reads=(), writes=(), **kw):
        return self.S.op("act", lambda e: e.activation(out, in_, func, **kw), reads, writes)

    def copy(self, eng, out, in_, reads=(), writes=()):
        if eng == "act":
            return self.S.op("act", lambda e: e.copy(out, in_), reads, writes)
        return self.S.op(eng, lambda e: e.tensor_copy(out, in_), reads, writes)

    def tt(self, eng, out, a, b, op, reads=(), writes=()):
        return self.S.op(eng, lambda e: e.tensor_tensor(out=out, in0=a, in1=b, op=op), reads, writes)

    def ts(self, eng, out, a, s1, s2, op0, op1=None, reads=(), writes=(), accum_out=None):
        if op1 is None:
            return self.S.op(eng, lambda e: e.tensor_scalar(out=out, in0=a, scalar1=s1, scalar2=None, op0=op0), reads, writes)
        if accum_out is not None:
            return self.S.op(eng, lambda e: e.tensor_scalar(out=out, in0=a, scalar1=s1, scalar2=s2, op0=op0, op1=op1,
                                                            accum_out=accum_out), reads, writes)
        return self.S.op(eng, lambda e: e.tensor_scalar(out=out, in0=a, scalar1=s1, scalar2=s2, op0=op0, op1=op1), reads, writes)

    def stt(self, out, in0, scalar, in1, op0, op1, reads=(), writes=(), accum_out=None):
        if accum_out is not None:
            return self.S.op("dve", lambda e: e.scalar_tensor_tensor(out=out, in0=in0, scalar=scalar, in1=in1, op0=op0,
                                                                     op1=op1, accum_out=accum_out), reads, writes)
        return self.S.op("dve", lambda e: e.scalar_tensor_tensor(out=out, in0=in0, scalar=scalar, in1=in1, op0=op0, op1=op1),
                         reads, writes)

    def rstd(self, rs, ss, n, t_ss, t_rs):
        self.act(rs, ss, AF.Ln, reads=[t_ss], writes=[t_rs], scale=1.0 / n, bias=self.eps_ap)
        self.act(rs, rs, AF.Exp, reads=[t_rs], writes=[t_rs], scale=-0.5)


def build(SEQ, CAP, dbg=False, stop=99):
    NT = SEQ // 128
    NCH = SEQ // 512
    NSLOT = NE * CAP
    TRASH = NSLOT
    CT = CAP // 128
    NH = CAP // 2
    assert NH <= 512 and CAP % 256 == 0 or CT == 1
    if CT == 1:
        NH = CAP
    NHALF = CAP // NH

    nc = bass.Bass("TRN2", target_bir_lowering=False)

    def din(name, shape, dt=F32):
        return nc.dram_tensor(name, list(shape), dt, kind="ExternalInput").ap()

    def dscr(name, shape, dt=F32):
        return nc.dram_tensor(name, list(shape), dt, kind="ExternalOutput" if dbg else "Internal").ap()

    x = din("x", [SEQ, D])
    mem = din("mem", [NMEM, D])
    g_mix = din("g_mix", [1, D])
    w_in = din("w_in", [D, 2048])
    g_sb = din("g_sb", [128, 4])
    g_pool = din("g_pool", [128, 4])
    w_pool = din("w_pool", [4, 128, 128])
    pool_scale = din("pool_scale", [128, 4])
    w_out = din("w_out", [D, D])
    g_mem_q = din("g_mem_q", [1, D])
    g_mem_kv = din("g_mem_kv", [1, D])
    w_mem_q = din("w_mem_q", [D, D])
    w_mem_kv = din("w_mem_kv", [D, 2 * D])
    w_mem_o = din("w_mem_o", [D, D])
    g_ffn = din("g_ffn", [1, D])
    w_router = din("w_router", [D, NE])
    b_router = din("b_router", [1, NE])
    w1 = din("w1", [NE, D, 2 * D])
    b1 = din("b1", [NE, 128, 16])
    w2 = din("w2", [NE, D, D])
    b2 = din("b2", [NE, 1, D])
    g_final = din("g_final", [1, D])
    c_ident = din("c_ident", [128, 128])
    c_tri1 = din("c_tri1", [128, 128])
    c_tri2 = din("c_tri2", [128, 128])
    c_ltri = din("c_ltri", [128, 128])
    c_ones = din("c_ones", [128, 128])
    c_mask = din("c_mask", [4, 128, 512])
    c_eoff = din("c_eoff", [128, NE])
    c_rcnt = din("c_rcnt", [128, 16])
    out = nc.dram_tensor("out", [SEQ, D], F32, kind="ExternalOutput").ap()

    uT_d = dscr("uT_d", [512, SEQ])
    sbT_d = dscr("sbT_d", [512, SEQ])
    yT_d = dscr("yT_d", [512, SEQ])
    x1_d = dscr("x1_d", [SEQ, D])
    x2_d = dscr("x2_d", [SEQ, D])
    xs_d = dscr("xs_d", [NSLOT + 128, D], BF16)
    ys_d = dscr("ys_d", [NSLOT + 128, D], BF16)
    pos_d = dscr("pos_d", [SEQ, 4], I32)
    gate_d = dscr("gate_d", [SEQ, 4])
    t_uT, t_sbT, t_yT, t_x1, t_x2, t_xs, t_ys, t_pos, t_gate = [Tok() for _ in range(9)]

    with contextlib.ExitStack() as top:
        def gsb(name, shape, dt):
            return top.enter_context(nc.sbuf_tensor(name, list(shape), dt))
        G = {"sems": {}, "eng_cnt": {e: 0 for e in Sched.ENGS}, "dma_cnt": [0] * N_DMA_SEMS}
        for e in Sched.ENGS:
            G["sems"][("eng", e)] = top.enter_context(nc.semaphore("s_" + e))
        for j in range(N_DMA_SEMS):
            G["sems"][("dma", j)] = top.enter_context(nc.semaphore("s_d%d" % j))
        eps_t = gsb("eps_t", [128, 1], F32)
        ident_f = gsb("ident_f", [128, 128], F32)
        ident_b = gsb("ident_b", [128, 128], BF16)
        ones_b = gsb("ones_b", [128, 128], BF16)
        KT = gsb("KT", [128, 8, NMEM], BF16)
        V = gsb("V", [128, 2, D], BF16)
        qkv = contextlib.ExitStack()
        qT = qkv.enter_context(nc.sbuf_tensor("qT", [128, 4, SEQ], BF16))
        kT = qkv.enter_context(nc.sbuf_tensor("kT", [128, 4, SEQ], BF16))
        vv = qkv.enter_context(nc.sbuf_tensor("vv", [128, NT, 512], BF16))

        with Phase(nc, "p0", G) as P:
            P.S.op("dve", lambda e: e.memset(eps_t[:], EPS))
            P.dma("sp", ident_f[:], c_ident)
            P.dma("pool", ident_b[:], c_ident)
            P.dma("pool", ones_b[:], c_ones)

        with (Phase(nc, "p1", G) if stop >= 1 else Skip()) as P:
            P.eps_ap = eps_t[:]
            w_bf, t_wl = P.sb([128, 8, 2048], BF16, "win", ntok=8)
            gbc, t_gbc = P.sb([128, D], F32, "gbc")
            for kc in range(8):
                P.dma("pool", w_bf[:, kc, :], w_in[kc * 128:(kc + 1) * 128, :], writes=[t_wl[kc]])
            P.dma("sp", gbc[:], g_mix.partition_broadcast(128), writes=[t_gbc])
            xt = [P.sb([128, D], F32, "x") for _ in range(3)]
            junks = [P.sb([128, D], BF16, "junk") for _ in range(2)]
            hb = [P.sb([128, D], BF16, "h") for _ in range(8)]
            ss = [P.sb([128, 1], F32, "ss") for _ in range(4)]
            hT = [P.sb([128, 8, 512], BF16, "hT") for _ in range(2)]
            ust = [P.sb([128, 512], F32, "ust") for _ in range(2)]
            ptr = [P.ps([128, 8, 128], BF16, "ptr") for _ in range(2)]
            pmm = [P.ps([128, 512], F32, "pmm") for _ in range(4)]
            st1 = {"mi": 0}

            def tileA1(c, t):
                ti = c * 4 + t
                tok0 = ti * 128
                xs_, t_x = xt[ti % 3]
                h_, t_h = hb[ti % 8]
                s_, t_s = ss[ti % 4]
                junk, t_junk = junks[ti % 2]
                P.dma("sp", xs_[:], x[tok0:tok0 + 128, :], writes=[t_x])
                yield
                P.act(junk[:], xs_[:], AF.Square, reads=[t_x], writes=[t_junk, t_s], accum_out=s_[:])
                yield
                P.act(s_[:], s_[:], AF.Ln, reads=[t_s], writes=[t_s], scale=1.0 / D, bias=eps_t[:])
                yield
                P.act(s_[:], s_[:], AF.Exp, reads=[t_s], writes=[t_s], scale=-0.5)
                yield
                P.stt(h_[:], xs_[:], s_[:, 0:1], gbc[:], ALU.mult, ALU.mult, reads=[t_x, t_s, t_gbc], writes=[t_h])
                yield

            def prepB1(c):
                hTc, t_hT = hT[c % 2]
                for t in range(4):
                    ti = c * 4 + t
                    h_, t_h = hb[ti % 8]
                    pt, t_pt = ptr[ti % 2]
                    for kc in range(8):
                        P.tr(pt[:, kc, :], h_[:, kc * 128:(kc + 1) * 128], ident_b[:], reads=[t_h], writes=[t_pt])
                    P.copy("dve" if t % 2 else "act", hTc[:, :, t * 128:(t + 1) * 128], pt[:], reads=[t_pt], writes=[t_hT])

            def mm1(c):
                hTc, t_hT = hT[c % 2]
                for j in range(12):
                    col = j * 128 if j < 8 else 1536 + (j - 8) * 128
                    pm, t_pm = pmm[st1["mi"] % 4]
                    st1["mi"] += 1
                    for kc in range(8):
                        P.mm(pm[:], w_bf[:, kc, col:col + 128], hTc[:, kc, :], kc == 0, kc == 7,
                             reads=[t_wl[kc], t_hT], writes=[t_pm])
                    if j < 4:
                        P.copy("act", qT[:, j, c * 512:(c + 1) * 512], pm[:], reads=[t_pm], writes=[])
                    elif j < 8:
                        P.copy("dve", kT[:, j - 4, c * 512:(c + 1) * 512], pm[:], reads=[t_pm], writes=[])
                    else:
                        us, t_us = ust[j % 2]
                        P.copy("act" if j % 2 else "dve", us[:], pm[:], reads=[t_pm], writes=[t_us])
                        P.dma("sp", uT_d[(j - 8) * 128:(j - 7) * 128, c * 512:(c + 1) * 512], us[:], reads=[t_us])
                for t in range(4):
                    pm, t_pm = pmm[st1["mi"] % 4]
                    st1["mi"] += 1
                    for kc in range(8):
                        P.mm(pm[:], hTc[:, kc, t * 128:(t + 1) * 128], w_bf[:, kc, 1024:1536], kc == 0, kc == 7,
                             reads=[t_wl[kc], t_hT], writes=[t_pm])
                    P.copy("act" if t % 2 else "dve", vv[:, c * 4 + t, :], pm[:], reads=[t_pm], writes=[])

            run_interleaved((tileA1(0, t) for t in range(4)), 2)
            prepB1(0)
            for c in range(NCH):
                if c + 1 < NCH:
                    run_interleaved((tileA1(c + 1, t) for t in range(4)), 2)
                mm1(c)
                if c + 1 < NCH:
                    prepB1(c + 1)

        with (Phase(nc, "p2", G) if stop >= 2 else Skip()) as P:
            tri1, t_c = P.sb([128, 128], BF16, "tri1")
            tri2, _ = P.sb([128, 128], BF16, "tri2")
            mask, _ = P.sb([128, 4, 2, 512], BF16, "mask")
            P.dma("pool", tri1[:], c_tri1, writes=[t_c])
            P.dma("pool", tri2[:], c_tri2, writes=[t_c])
            for j in range(4):
                for s_ in range(2):
                    P.dma("pool", mask[:, j, s_, :], c_mask[j], writes=[t_c])
            NB = 3
            Eb = [P.sb([128, 2, 512], F32, "E") for _ in range(NB)]
            Lb = [P.sb([128, 2, 512], BF16, "L") for _ in range(NB)]
            Xb = [P.sb([128, 2, 512], F32, "X") for _ in range(NB)]
            Wb = [P.sb([128, 2, 512], BF16, "W") for _ in range(NB)]
            Ob = [P.sb([64, 512], F32, "O") for _ in range(2)]
            Zp = [P.ps([128, 2, 512], F32, "Z") for _ in range(2)]
            Gp, t_g = P.ps([128, 2, 512], F32, "G")
            Op_ = [P.ps([64, 512], F32, "Oa") for _ in range(2)]
            class Seg:
                def __init__(self, hp, qc, n0):
                    self.hp, self.qc, self.n0 = hp, qc, n0
                    self.nb = 4 * qc + 4
                    self.q0 = qc * 512

                def diag(self, i):
                    return (self.nb - 1 - i) >= 4 * self.qc

                def cs(self, i):
                    j = self.nb - 1 - i - 4 * self.qc
                    return slice(128 * j, 512) if j > 0 else slice(0, 512)

                def qk(self, i):
                    kb = self.nb - 1 - i
                    z, t_z = Zp[(self.n0 + i) % 2]
                    c = self.cs(i)
                    for s in range(2):
                        pb = s * 64
                        P.mm(z[:, s, c], kT[pb:pb + 64, self.hp, kb * 128:(kb + 1) * 128],
                             qT[pb:pb + 64, self.hp, self.q0 + c.start:self.q0 + 512], True, True, writes=[t_z])

                def front(self, i):
                    (z, t_z), (E, t_E) = Zp[(self.n0 + i) % 2], Eb[(self.n0 + i) % NB]
                    c = self.cs(i)
                    P.act(E[:, :, c], z[:, :, c], AF.Exp, reads=[t_z], writes=[t_E], scale=0.125)

                def midA(self, i):
                    k = (self.n0 + i) % NB
                    (E, t_E), (L, t_L) = Eb[k], Lb[k]
                    c = self.cs(i)
                    P.act(L[:, :, c], E[:, :, c], AF.Ln, reads=[t_E], writes=[t_L], bias=1.0)
                    if self.diag(i):
                        j = self.nb - 1 - i - 4 * self.qc
                        P.tt("dve", L[:, :, c], L[:, :, c], mask[:, j, :, c], ALU.mult, reads=[t_L, t_c], writes=[t_L])

                def midB(self, i):
                    L, t_L = Lb[(self.n0 + i) % NB]
                    c = self.cs(i)
                    for s in range(2):
                        P.mm(Gp[:, s, c], tri1[:], L[:, s, c], i == 0, False, reads=[t_L, t_c], writes=[t_g])

                def backA(self, i):
                    k = (self.n0 + i) % NB
                    (L, t_L), (X, t_X) = Lb[k], Xb[k]
                    c = self.cs(i)
                    P.act(X[:, :, c], Gp[:, :, c], AF.Exp, reads=[t_g], writes=[t_X])
                    if i + 1 < self.nb:
                        for s in range(2):
                            P.mm(Gp[:, s, c], tri2[:], L[:, s, c], False, False, reads=[t_L, t_c], writes=[t_g])

                def backB(self, i):
                    k = (self.n0 + i) % NB
                    (E, t_E), (X, t_X), (W, t_W) = Eb[k], Xb[k], Wb[k]
                    c = self.cs(i)
                    P.tt("dve", W[:, :, c], E[:, :, c], X[:, :, c], ALU.mult, reads=[t_E, t_X], writes=[t_W])
                    if self.diag(i):
                        j = self.nb - 1 - i - 4 * self.qc
                        P.tt("dve", W[:, :, c], W[:, :, c], mask[:, j, :, c], ALU.mult, reads=[t_W, t_c], writes=[t_W])

                def pv(self, i):
                    kb = self.nb - 1 - i
                    W, t_W = Wb[(self.n0 + i) % NB]
                    c = self.cs(i)
                    for s in range(2):
                        o, t_o = Op_[s]
                        h = self.hp * 2 + s
                        P.mm(o[0:64, c], vv[:, kb, h * 64:(h + 1) * 64], W[:, s, c], i == 0, i == self.nb - 1,
                             reads=[t_W], writes=[t_o])

                def tail(self):
                    last = self.nb - 1
                    self.backB(last)
                    self.pv(last)
                    for s in range(2):
                        o, t_o = Op_[s]
                        ob, t_ob = Ob[s]
                        h = self.hp * 2 + s
                        P.copy("dve", ob[:], o[0:64, :], reads=[t_o], writes=[t_ob])
                        P.dma("sp", sbT_d[h * 64:(h + 1) * 64, self.q0:self.q0 + 512], ob[:], reads=[t_ob])

            segs = []
            n0 = 0
            for hp in range(4):
                for qc in range(NCH):
                    segs.append(Seg(hp, qc, n0))
                    n0 += segs[-1].nb
            segs[0].qk(0)
            prev = None
            for k, F in enumerate(segs):
                for i in range(F.nb):
                    F.front(i)
                    if i + 1 < F.nb:
                        F.qk(i + 1)
                    elif k + 1 < len(segs):
                        segs[k + 1].qk(0)
                    dg = F.diag(i)
                    if dg:
                        F.midA(i)
                    if i > 0:
                        F.backA(i - 1)
                    elif prev is not None:
                        prev.backA(prev.nb - 1)
                    if not dg:
                        F.midA(i)
                    F.midB(i)
                    if i > 0:
                        F.backB(i - 1)
                        F.pv(i - 1)
                    elif prev is not None:
                        prev.tail()
                prev = F
            prev.backA(prev.nb - 1)
            prev.tail()

        qkv.close()

        with (Phase(nc, "p3", G) if stop >= 3 else Skip()) as P:
            wp, t_wp = P.sb([128, 4, 128], BF16, "wp")
            psc, t_psc = P.sb([128, 4], F32, "psc")
            rc, t_rc = P.sb([128, 16], F32, "rc")
            for g in range(4):
                P.dma("pool", wp[:, g, :], w_pool[g], writes=[t_wp])
            P.dma("sp", psc[:], pool_scale, writes=[t_psc])
            P.dma("sp", rc[:], c_rcnt, writes=[t_rc])
            ub = [P.sb([128, SEQ], F32, "u") for _ in range(4)]
            wa2 = [[P.sb([128, SEQ], F32, "wa") for _ in range(2)] for _ in range(2)]
            pl = [P.sb([128, SEQ], BF16, "pl") for _ in range(2)]
            yst = [P.sb([128, 512], F32, "y") for _ in range(2)]
            pp = [P.ps([128, 512], F32, "pp") for _ in range(2)]
            k = 0
            for g in range(4):
                W = 2 << g
                u, t_u = ub[g]
                P.dma("act", u[:], uT_d[g * 128:(g + 1) * 128, :], writes=[t_u])
                eng = "dve" if g % 2 == 0 else "pool"
                wa = wa2[g % 2]
                src, t_src = u, t_u
                sh = 1
                step = 0
                while sh < W:
                    dst, t_dst = wa[step % 2]
                    P.tt(eng, dst[:, sh:SEQ], src[:, sh:SEQ], src[:, 0:SEQ - sh], ALU.add, reads=[t_src], writes=[t_dst])
                    P.copy(eng, dst[:, 0:sh], src[:, 0:sh], reads=[t_src], writes=[t_dst])
                    src, t_src = dst, t_dst
                    sh *= 2
                    step += 1
                p_, t_p = pl[g % 2]
                P.stt(p_[:, W:SEQ], src[:, W:SEQ], 1.0 / W, u[:, W:SEQ], ALU.mult, ALU.subtract,
                      reads=[t_src, t_u], writes=[t_p])
                tmp, t_tmp = wa[step % 2]
                P.tt("dve", tmp[:, 0:W], src[:, 0:W], rc[:, 0:W], ALU.mult, reads=[t_src, t_rc], writes=[t_tmp])
                P.tt("dve", p_[:, 0:W], tmp[:, 0:W], u[:, 0:W], ALU.subtract, reads=[t_tmp, t_u], writes=[t_p])
                for c in range(NCH):
                    pm, t_pm = pp[k % 2]
                    y, t_y = yst[k % 2]
                    k += 1
                    P.mm(pm[:], wp[:, g, :], p_[:, c * 512:(c + 1) * 512], True, True, reads=[t_wp, t_p], writes=[t_pm])
                    P.ts("dve", y[:], pm[:], psc[:, g:g + 1], None, ALU.mult, reads=[t_pm, t_psc], writes=[t_y])
                    P.dma("sp", yT_d[g * 128:(g + 1) * 128, c * 512:(c + 1) * 512], y[:], reads=[t_y])

        pre5 = contextlib.ExitStack()
        wq = pre5.enter_context(nc.sbuf_tensor("wq_pre", [128, 8, D], BF16))
        wo = pre5.enter_context(nc.sbuf_tensor("wo_pre", [128, 8, D], BF16))

        with (Phase(nc, "p4", G) if stop >= 4 else Skip()) as P:
            P.eps_ap = eps_t[:]
            wos, t_wosl = P.sb([128, 4, D], BF16, "wos", ntok=4)
            wop, t_wopl = P.sb([128, 4, D], BF16, "wop", ntok=4)
            gs, t_gs = P.sb([128, 4], F32, "gs")
            gp, t_gp = P.sb([128, 4], F32, "gp")
            for h in range(4):
                P.dma("pool", wos[:, h, :], w_out[h * 128:(h + 1) * 128, :], writes=[t_wosl[h]])
            for g in range(4):
                P.dma("pool", wop[:, g, :], w_out[512 + g * 128:512 + (g + 1) * 128, :], writes=[t_wopl[g]])
            P.dma("sp", gs[:], g_sb, writes=[t_gs])
            P.dma("sp", gp[:], g_pool, writes=[t_gp])

            def prefetch5():
                for kc in range(8):
                    P.dma("pool", wq[:, kc, :], w_mem_q[kc * 128:(kc + 1) * 128, :])
                for kc in range(8):
                    P.dma("pool", wo[:, kc, :], w_mem_o[kc * 128:(kc + 1) * 128, :])
            sbc = [P.sb([128, 4, 512], F32, "sbc") for _ in range(2)]
            yc = [P.sb([128, 4, 512], F32, "yc") for _ in range(2)]
            asb = [P.sb([128, 4, 512], BF16, "asb", ntok=4) for _ in range(2)]
            apl = [P.sb([128, 4, 512], BF16, "apl", ntok=4) for _ in range(2)]
            sqs = [P.sb([128, 4, 512], BF16, "sqs") for _ in range(2)]
            sqp = [P.sb([128, 4, 512], BF16, "sqp") for _ in range(2)]
            xt = [P.sb([128, D], F32, "x") for _ in range(2)]
            ot = [P.sb([128, D], F32, "o") for _ in range(2)]
            rs = [P.sb([128, 2], F32, "rs") for _ in range(2)]
            pss = [P.ps([128, 2], F32, "pss") for _ in range(2)]
            po = [P.ps([128, D], F32, "po") for _ in range(2)]

            def prep4(c):
                (sc, t_sc), (y_, t_y), (a_s, t_as), (a_p, t_ap) = sbc[c % 2], yc[c % 2], asb[c % 2], apl[c % 2]
                (q_s, t_qs), (q_p, t_qp) = sqs[c % 2], sqp[c % 2]
                P.dma("sp", sc[:], sbT_d[:, c * 512:(c + 1) * 512].rearrange("(h d) t -> d h t", d=128), writes=[t_sc])
                P.dma("sp", y_[:], yT_d[:, c * 512:(c + 1) * 512].rearrange("(g e) t -> e g t", e=128), writes=[t_y])
                P.act(q_s[:], sc[:], AF.Square, reads=[t_sc], writes=[t_qs])
                P.act(q_p[:], y_[:], AF.Square, reads=[t_y], writes=[t_qp])
                for h in range(4):
                    if h % 2:
                        P.ts("dve", a_s[:, h, :], sc[:, h, :], gs[:, h:h + 1], None, ALU.mult, reads=[t_sc, t_gs], writes=[t_as[h]])
                    else:
                        P.act(a_s[:, h, :], sc[:, h, :], AF.Copy, reads=[t_sc, t_gs], writes=[t_as[h]], scale=gs[:, h:h + 1])
                for g in range(4):
                    if g % 2:
                        P.ts("dve", a_p[:, g, :], y_[:, g, :], gp[:, g:g + 1], None, ALU.mult, reads=[t_y, t_gp], writes=[t_ap[g]])
                    else:
                        P.act(a_p[:, g, :], y_[:, g, :], AF.Copy, reads=[t_y, t_gp], writes=[t_ap[g]], scale=gp[:, g:g + 1])

            def tile4(c, t):
                (a_s, t_as), (a_p, t_ap) = asb[c % 2], apl[c % 2]
                (q_s, t_qs), (q_p, t_qp) = sqs[c % 2], sqp[c % 2]
                ti = c * 4 + t
                tok0 = ti * 128
                tsl = slice(t * 128, (t + 1) * 128)
                xs_, t_x = xt[ti % 2]
                o_, t_o = ot[ti % 2]
                r_, t_r = rs[ti % 2]
                ps_, t_ps = pss[ti % 2]
                pz, t_pz = po[ti % 2]
                P.dma("sp", xs_[:], x[tok0:tok0 + 128, :], writes=[t_x])
                for h in range(4):
                    P.mm(ps_[:, 0:1], q_s[:, h, tsl], ones_b[:, 0:1], h == 0, h == 3, reads=[t_qs], writes=[t_ps])
                yield
                P.act(r_[:, 0:1], ps_[:, 0:1], AF.Ln, reads=[t_ps], writes=[t_r], scale=1.0 / 512, bias=eps_t[:])
                yield
                for g in range(4):
                    P.mm(ps_[:, 1:2], q_p[:, g, tsl], ones_b[:, 0:1], g == 0, g == 3, reads=[t_qp], writes=[t_ps])
                yield
                P.act(r_[:, 1:2], ps_[:, 1:2], AF.Ln, reads=[t_ps], writes=[t_r], scale=1.0 / 512, bias=eps_t[:])
                yield
                P.act(r_[:], r_[:], AF.Exp, reads=[t_r], writes=[t_r], scale=-0.5)
                for nh in range(2):
                    for h in range(4):
                        P.mm(pz[:, nh * 512:(nh + 1) * 512], a_s[:, h, tsl], wos[:, h, nh * 512:(nh + 1) * 512],
                             h == 0, h == 3, reads=[t_as[h], t_wosl[h]], writes=[t_pz])
                yield
                for nh in range(2):
                    hs = slice(nh * 512, (nh + 1) * 512)
                    P.stt(o_[:, hs], pz[:, hs], r_[:, 0:1], xs_[:, hs], ALU.mult, ALU.add, reads=[t_pz, t_r, t_x], writes=[t_o])
                yield
                for nh in range(2):
                    for g in range(4):
                        P.mm(pz[:, nh * 512:(nh + 1) * 512], a_p[:, g, tsl], wop[:, g, nh * 512:(nh + 1) * 512],
                             g == 0, g == 3, reads=[t_ap[g], t_wopl[g]], writes=[t_pz])
                yield
                for nh in range(2):
                    hs = slice(nh * 512, (nh + 1) * 512)
                    P.stt(o_[:, hs], pz[:, hs], r_[:, 1:2], o_[:, hs], ALU.mult, ALU.add, reads=[t_pz, t_r, t_o], writes=[t_o])
                yield
                P.dma("sp", x1_d[tok0:tok0 + 128, :], o_[:], reads=[t_o])
                yield

            prep4(0)
            for c in range(NCH):
                if c == min(2, NCH - 1):
                    prefetch5()
                if c + 1 < NCH:
                    prep4(c + 1)
                run_interleaved((tile4(c, t) for t in range(4)), 2)

        with (Phase(nc, "p5a", G) if stop >= 5 else Skip()) as P:
            P.eps_ap = eps_t[:]
            junk, t_junk = P.sb([128, D], BF16, "junk")
            ptr = [P.ps([128, 8, 128], BF16, "ptr") for _ in range(2)]
            pmm = [P.ps([128, 512], F32, "pmm") for _ in range(3)]
            wkv, t_wkvl = P.sb([128, 8, 2 * D], BF16, "wkv", ntok=8)
            gkv, t_gkv = P.sb([128, D], F32, "gkv")
            mT, t_mT = P.sb([128, 8, NMEM], BF16, "mT")
            mt = [P.sb([128, D], F32, "m") for _ in range(2)]
            mb = [P.sb([128, D], BF16, "mb") for _ in range(2)]
            mss = [P.sb([128, 1], F32, "mss") for _ in range(2)]
            for kc in range(8):
                P.dma("pool", wkv[:, kc, :], w_mem_kv[kc * 128:(kc + 1) * 128, :], writes=[t_wkvl[kc]])
            P.dma("sp", gkv[:], g_mem_kv.partition_broadcast(128), writes=[t_gkv])
            for t in range(2):
                (m_, t_m), (b_, t_b), (s_, t_s) = mt[t], mb[t], mss[t]
                pt, t_pt = ptr[t]
                P.dma("sp", m_[:], mem[t * 128:(t + 1) * 128, :], writes=[t_m])
                P.act(junk[:], m_[:], AF.Square, reads=[t_m], writes=[t_junk, t_s], accum_out=s_[:])
                P.rstd(s_[:], s_[:], D, t_s, t_s)
                P.stt(b_[:], m_[:], s_[:, 0:1], gkv[:], ALU.mult, ALU.mult, reads=[t_m, t_s, t_gkv], writes=[t_b])
                for kc in range(8):
                    P.tr(pt[:, kc, :], b_[:, kc * 128:(kc + 1) * 128], ident_b[:], reads=[t_b], writes=[t_pt])
                P.copy("dve", mT[:, :, t * 128:(t + 1) * 128], pt[:], reads=[t_pt], writes=[t_mT])
            mi = 0
            for fc in range(8):
                pm, t_pm = pmm[mi % 3]
                mi += 1
                for kc in range(8):
                    P.mm(pm[:, 0:NMEM], wkv[:, kc, fc * 128:(fc + 1) * 128], mT[:, kc, :], kc == 0, kc == 7,
                         reads=[t_wkvl[kc], t_mT], writes=[t_pm])
                P.copy("act" if fc % 2 else "dve", KT[:, fc, :], pm[:, 0:NMEM], reads=[t_pm], writes=[])
            for t in range(2):
                for nh in range(2):
                    pm, t_pm = pmm[mi % 3]
                    mi += 1
                    for kc in range(8):
                        P.mm(pm[:], mT[:, kc, t * 128:(t + 1) * 128], wkv[:, kc, D + nh * 512:D + (nh + 1) * 512],
                             kc == 0, kc == 7, reads=[t_wkvl[kc], t_mT], writes=[t_pm])
                    P.copy("act" if nh else "dve", V[:, t, nh * 512:(nh + 1) * 512], pm[:], reads=[t_pm], writes=[])

        with (Phase(nc, "p5", G) if stop >= 5 else Skip()) as P:
            P.eps_ap = eps_t[:]
            t_wql = [Tok() for _ in range(8)]
            t_wol = [Tok() for _ in range(8)]
            gq, t_gq = P.sb([128, D], F32, "gq")
            P.dma("sp", gq[:], g_mem_q.partition_broadcast(128), writes=[t_gq])
            ptr = [P.ps([128, 8, 128], BF16, "ptr") for _ in range(2)]
            pmm = [P.ps([128, 512], F32, "pmm") for _ in range(3)]
            psc_ = [P.ps([128, NMEM], F32, "psc") for _ in range(3)]
            NU = 5
            junks = [P.sb([128, D], BF16, "junk") for _ in range(2)]
            xc = [P.sb([128, 4, D], F32, "x1c") for _ in range(2)]
            hq = [P.sb([128, D], BF16, "hq") for _ in range(8)]
            sq = [P.sb([128, 1], F32, "sq") for _ in range(4)]
            hqTb = [P.sb([128, 8, 512], BF16, "hqT") for _ in range(2)]
            qmb = [P.sb([128, 8, 512], BF16, "qm", ntok=8) for _ in range(2)]
            pf = [P.sb([128, NMEM], F32, "pf") for _ in range(NU)]
            pb = [P.sb([128, NMEM], BF16, "pb") for _ in range(NU)]
            sm = [P.sb([128, 4], F32, "sm") for _ in range(NU)]
            pT, t_pTl = P.sb([128, 2, 4, 512], BF16, "pT", ntok=4)
            oT, t_oTl = P.sb([128, 8, 512], BF16, "oT", ntok=8)
            x2t = [P.sb([128, D], F32, "x2") for _ in range(2)]
            st5 = {"mi": 0}

            def load5(c):
                xc_, t_xc = xc[c % 2]
                P.dma("sp", xc_[:], x1_d[c * 512:(c + 1) * 512, :].rearrange("(t p) d -> p t d", p=128), writes=[t_xc])

            def tileA(c, t):
                xc_, t_xc = xc[c % 2]
                ti = c * 4 + t
                (h_, t_h), (s_, t_s) = hq[ti % 8], sq[ti % 4]
                junk, t_junk = junks[ti % 2]
                P.act(junk[:], xc_[:, t, :], AF.Square, reads=[t_xc], writes=[t_junk, t_s], accum_out=s_[:])
                yield
                P.act(s_[:], s_[:], AF.Ln, reads=[t_s], writes=[t_s], scale=1.0 / D, bias=eps_t[:])
                yield
                P.act(s_[:], s_[:], AF.Exp, reads=[t_s], writes=[t_s], scale=-0.5)
                yield
                P.stt(h_[:], xc_[:, t, :], s_[:, 0:1], gq[:], ALU.mult, ALU.mult, reads=[t_xc, t_s, t_gq], writes=[t_h])
                yield

            def trA(c):
                hqT, t_hqT = hqTb[c % 2]
                for t in range(4):
                    ti = c * 4 + t
                    h_, t_h = hq[ti % 8]
                    pt, t_pt = ptr[ti % 2]
                    for kc in range(8):
                        P.tr(pt[:, kc, :], h_[:, kc * 128:(kc + 1) * 128], ident_b[:], reads=[t_h], writes=[t_pt])
                    P.copy("dve" if t % 2 else "act", hqT[:, :, t * 128:(t + 1) * 128], pt[:], reads=[t_pt], writes=[t_hqT])

            def unitC(c, t, hd):
                k = (c * 4 + t) * 4 + hd
                qm, t_qml = qmb[c % 2]
                tsl = slice(t * 128, (t + 1) * 128)
                pscr, t_pscr = psc_[k % 3]
                (p_f, t_pf), (p_b, t_pb), (m_, t_m) = pf[k % NU], pb[k % NU], sm[k % NU]
                pt, t_pt = ptr[k % 2]
                for dc in range(2):
                    P.mm(pscr[:, 0:NMEM], qm[:, hd * 2 + dc, tsl], KT[:, hd * 2 + dc, :], dc == 0, dc == 1,
                         reads=[t_qml[hd * 2 + dc]], writes=[t_pscr])
                P.S.op("dve", lambda e: e.reduce_max(out=m_[:, 0:1], in_=pscr[:, 0:NMEM], axis=mybir.AxisListType.X),
                       [t_pscr], [t_m])
                P.ts("dve", m_[:, 1:2], m_[:, 0:1], -1.0 / 16, None, ALU.mult, reads=[t_m], writes=[t_m])
                P.act(p_f[:], pscr[:, 0:NMEM], AF.Exp, reads=[t_pscr, t_m], writes=[t_pf, t_m], scale=1.0 / 16,
                      bias=m_[:, 1:2], accum_out=m_[:, 2:3])
                yield
                P.S.op("dve", lambda e: e.reciprocal(m_[:, 3:4], m_[:, 2:3]), [t_m], [t_m])
                yield
                P.ts("dve", p_b[:], p_f[:], m_[:, 3:4], None, ALU.mult, reads=[t_pf, t_m], writes=[t_pb])
                yield
                for mc in range(2):
                    P.tr(pt[:, mc, :], p_b[:, mc * 128:(mc + 1) * 128], ident_b[:], reads=[t_pb], writes=[t_pt])
                P.copy("act", pT[:, :, hd, tsl], pt[:, 0:2, :], reads=[t_pt], writes=[t_pTl[hd]])
                yield

            def projB(c, fc):
                hqT, t_hqT = hqTb[c % 2]
                qm, t_qml = qmb[c % 2]
                pm, t_pm = pmm[st5["mi"] % 3]
                st5["mi"] += 1
                for kc in range(8):
                    P.mm(pm[:], wq[:, kc, fc * 128:(fc + 1) * 128], hqT[:, kc, :], kc == 0, kc == 7,
                         reads=[t_wql[kc], t_hqT], writes=[t_pm])
                P.copy("act" if fc % 2 else "dve", qm[:, fc, :], pm[:], reads=[t_pm], writes=[t_qml[fc]])

            def unitsC(c):
                gens = [unitC(c, t, hd) for t in range(4) for hd in range(4)]
                active = []
                nxt = 0
                fc = 0
                rounds = 0
                while nxt < len(gens) or active:
                    while nxt < len(gens) and len(active) < 4:
                        active.append(gens[nxt])
                        nxt += 1
                    for g_ in list(active):
                        try:
                            next(g_)
                        except StopIteration:
                            active.remove(g_)
                    rounds += 1
                    if c + 1 < NCH and fc < 8 and rounds % 2 == 0:
                        projB(c + 1, fc)
                        fc += 1
                while c + 1 < NCH and fc < 8:
                    projB(c + 1, fc)
                    fc += 1

            load5(0)
            run_interleaved((tileA(0, t) for t in range(4)), 2)
            trA(0)
            for fc in range(8):
                projB(0, fc)
            for c in range(NCH):
                xc_, t_xc = xc[c % 2]
                if c + 1 < NCH:
                    load5(c + 1)
                    run_interleaved((tileA(c + 1, t) for t in range(4)), 2)
                    trA(c + 1)
                unitsC(c)
                for fc in range(8):
                    hd = fc // 2
                    pm, t_pm = pmm[st5["mi"] % 3]
                    st5["mi"] += 1
                    for mc in range(2):
                        P.mm(pm[:], V[:, mc, fc * 128:(fc + 1) * 128], pT[:, mc, hd, :], mc == 0, mc == 1,
                             reads=[t_pTl[hd]], writes=[t_pm])
                    P.copy("act" if fc % 2 else "dve", oT[:, fc, :], pm[:], reads=[t_pm], writes=[t_oTl[fc]])
                for t in range(4):
                    tok0 = c * 512 + t * 128
                    tsl = slice(t * 128, (t + 1) * 128)
                    x2_, t_x2t = x2t[t % 2]
                    for nh in range(2):
                        pm, t_pm = pmm[st5["mi"] % 3]
                        st5["mi"] += 1
                        for fc in range(8):
                            P.mm(pm[:], oT[:, fc, tsl], wo[:, fc, nh * 512:(nh + 1) * 512], fc == 0, fc == 7,
                                 reads=[t_oTl[fc], t_wol[fc]], writes=[t_pm])
                        P.tt("dve", x2_[:, nh * 512:(nh + 1) * 512], pm[:], xc_[:, t, nh * 512:(nh + 1) * 512], ALU.add,
                             reads=[t_pm, t_xc], writes=[t_x2t])
                    P.dma("sp", x2_d[tok0:tok0 + 128, :], x2_[:], reads=[t_x2t])

        pre5.close()
        pre7 = contextlib.ExitStack()
        w1b0 = pre7.enter_context(nc.sbuf_tensor("w1b_pre", [128, 8, 2 * D], BF16))
        w2b0 = pre7.enter_context(nc.sbuf_tensor("w2b_pre", [128, 8, D], BF16))
        b1t0 = pre7.enter_context(nc.sbuf_tensor("b1t_pre", [128, 16], F32))
        b2t0 = pre7.enter_context(nc.sbuf_tensor("b2t_pre", [128, D], F32))

        with (Phase(nc, "p6", G) if stop >= 6 else Skip()) as P:
            P.eps_ap = eps_t[:]
            wr, t_wr = P.sb([128, 8, NE], F32, "wr")
            br, t_br = P.sb([128, NE], F32, "br")
            gf, t_gf = P.sb([128, D], F32, "gf")
            eoff, t_eo = P.sb([128, NE], F32, "eoff")
            ltri, t_lt = P.sb([128, 128], BF16, "ltri")
            cum, t_cum = P.sb([128, NE], F32, "cum")
            cumb, t_cumb = P.sb([128, NE], BF16, "cumb")
            P.dma("sp", wr[:], w_router.rearrange("(kc p) e -> p kc e", p=128), writes=[t_wr])
            P.dma("sp", br[:], b_router.partition_broadcast(128), writes=[t_br])
            P.dma("sp", gf[:], g_ffn.partition_broadcast(128), writes=[t_gf])
            P.dma("sp", eoff[:], c_eoff, writes=[t_eo])
            P.dma("pool", ltri[:], c_ltri, writes=[t_lt])

            def prefetch7():
                for kc in range(8):
                    P.dma("pool", w1b0[:, kc, :], w1[0, kc * 128:(kc + 1) * 128, :])
                for kc in range(8):
                    P.dma("pool", w2b0[:, kc, :], w2[0, kc * 128:(kc + 1) * 128, :])
                P.dma("sp", b1t0[:], b1[0])
                P.dma("sp", b2t0[:], b2[0].partition_broadcast(128))
            P.S.op("dve", lambda e: e.memset(cum[:], 0.0), [], [t_cum])
            P.S.op("dve", lambda e: e.memset(cumb[:], 0.0), [], [t_cumb])
            NB6 = 4
            junks = [P.sb([128, D], BF16, "junk") for _ in range(NB6)]
            xt = [P.sb([128, D], F32, "x") for _ in range(NB6)]
            hf = [P.sb([128, D], F32, "hf") for _ in range(NB6)]
            hbb = [P.sb([128, D], BF16, "hb") for _ in range(NB6)]
            hT3 = [P.sb([128, 8, 128], F32, "hT3") for _ in range(NB6)]
            sq = [P.sb([128, 1], F32, "sq") for _ in range(NB6)]
            lg = [P.sb([128, NE], F32, "lg") for _ in range(NB6)]
            t8 = [P.sb([128, 8], F32, "t8") for _ in range(NB6)]
            sel = [P.sb([128, NE], BF16, "sel") for _ in range(NB6)]
            self_ = [P.sb([128, NE], F32, "self") for _ in range(NB6)]
            posf = [P.sb([128, NE], F32, "posf") for _ in range(NB6)]
            sc32 = [P.sb([128, NE], F32, "sc32") for _ in range(NB6)]
            pk = [P.sb([128, 4], F32, "pk") for _ in range(NB6)]
            pki = [P.sb([128, 4], I32, "pki") for _ in range(NB6)]
            gt = [P.sb([128, 8], F32, "gt") for _ in range(NB6)]
            ptr = [P.ps([128, 8, 128], F32, "ptr") for _ in range(2)]
            plg = [P.ps([128, NE], F32, "plg") for _ in range(2)]
            prk = [P.ps([128, NE], F32, "prk") for _ in range(2)]

            def tile6(ti):
                tok0 = ti * 128
                b = ti % NB6
                (x_, t_x), (h_, t_h), (hb_, t_hb), (hT_, t_hT), (s_, t_s) = xt[b], hf[b], hbb[b], hT3[b], sq[b]
                (l_, t_l), (t8_, t_t8), (se_, t_se), (sf_, t_sf), (po_, t_po), (sc_, t_scr) = lg[b], t8[b], sel[b], self_[b], posf[b], sc32[b]
                (pk_, t_pk), (pki_, t_pki), (g_, t_g) = pk[b], pki[b], gt[b]
                (pt, t_pt), (pl_, t_pl), (pr_, t_pr) = ptr[ti % 2], plg[ti % 2], prk[ti % 2]
                junk, t_junk = junks[b]
                if ti == min(8, NT - 1):
                    prefetch7()
                P.dma("act", x_[:], x2_d[tok0:tok0 + 128, :], writes=[t_x])
                yield
                P.act(junk[:], x_[:], AF.Square, reads=[t_x], writes=[t_junk, t_s], accum_out=s_[:])
                yield
                P.act(s_[:], s_[:], AF.Ln, reads=[t_s], writes=[t_s], scale=1.0 / D, bias=eps_t[:])
                yield
                P.act(s_[:], s_[:], AF.Exp, reads=[t_s], writes=[t_s], scale=-0.5)
                yield
                P.stt(h_[:], x_[:], s_[:, 0:1], gf[:], ALU.mult, ALU.mult, reads=[t_x, t_s, t_gf], writes=[t_h])
                yield
                P.copy("act", hb_[:], h_[:], reads=[t_h], writes=[t_hb])
                for kc in range(8):
                    P.tr(pt[:, kc, :], h_[:, kc * 128:(kc + 1) * 128], ident_f[:], reads=[t_h], writes=[t_pt])
                P.copy("dve", hT_[:, 0:4, :], pt[:, 0:4, :], reads=[t_pt], writes=[t_hT])
                P.copy("act", hT_[:, 4:8, :], pt[:, 4:8, :], reads=[t_pt], writes=[t_hT])
                yield
                for kc in range(8):
                    P.mm(pl_[:, 0:NE], hT_[:, kc, :], wr[:, kc, :], kc == 0, kc == 7, reads=[t_hT, t_wr], writes=[t_pl])
                P.tt("dve", l_[:], pl_[:, 0:NE], br[:], ALU.add, reads=[t_pl, t_br], writes=[t_l])
                yield
                P.S.op("dve", lambda e, t8_=t8_, l_=l_: e.max(t8_[:], l_[:]), [t_l], [t_t8])
                yield
                P.ts("dve", sf_[:], l_[:], t8_[:, 3:4], None, ALU.is_ge, reads=[t_l, t_t8], writes=[t_sf])
                P.ts("pool", g_[:, 4:5], t8_[:, 0:1], -1.0, None, ALU.mult, reads=[t_t8], writes=[t_g])
                yield
                P.copy("dve", se_[:], sf_[:], reads=[t_sf], writes=[t_se])
                P.act(g_[:, 0:4], t8_[:, 0:4], AF.Exp, reads=[t_t8, t_g], writes=[t_g], bias=g_[:, 4:5], accum_out=g_[:, 5:6])
                yield
                P.mm(pr_[:, 0:NE], ltri[:], se_[:], True, False, reads=[t_lt, t_se], writes=[t_pr])
                P.mm(pr_[:, 0:NE], ones_b[:], cumb[:], False, True, reads=[t_cumb], writes=[t_pr])
                P.tt("dve", cum[:], cum[:], sf_[:], ALU.add, reads=[t_cum, t_sf], writes=[t_cum])
                P.copy("dve", cumb[:], cum[:], reads=[t_cum], writes=[t_cumb])
                P.ts("dve", sc_[:], pr_[:, 0:NE], float(CAP), 1.0e6, ALU.is_ge, ALU.mult, reads=[t_pr], writes=[t_scr])
                P.tt("dve", po_[:], pr_[:, 0:NE], eoff[:], ALU.add, reads=[t_pr, t_eo], writes=[t_po])
                yield
                P.S.op("dve", lambda e, g_=g_: e.reciprocal(g_[:, 6:7], g_[:, 5:6]), [t_g], [t_g])
                yield
                P.ts("dve", g_[:, 0:4], g_[:, 0:4], g_[:, 6:7], None, ALU.mult, reads=[t_g], writes=[t_g])
                yield
                P.dma("sp", gate_d[tok0:tok0 + 128, :], g_[:, 0:4], reads=[t_g])
                P.tt("dve", po_[:], po_[:], sc_[:], ALU.add, reads=[t_po, t_scr], writes=[t_po])
                yield
                P.ts("dve", po_[:], po_[:], float(TRASH), None, ALU.min, reads=[t_po], writes=[t_po])
                yield
                for k in range(4):
                    P.stt(sc_[:], l_[:], t8_[:, k:k + 1], po_[:], ALU.is_equal, ALU.mult, reads=[t_l, t_t8, t_po],
                          writes=[t_scr, t_pk], accum_out=pk_[:, k:k + 1])
                    yield
                P.copy("dve", pki_[:], pk_[:], reads=[t_pk], writes=[t_pki])
                yield
                P.dma("sp", pos_d[tok0:tok0 + 128, :], pki_[:], reads=[t_pki])
                for k in range(4):
                    P.S.dma("pool", lambda e, pki_=pki_, hb_=hb_, k=k: e.indirect_dma_start(
                        out=xs_d, out_offset=bass.IndirectOffsetOnAxis(ap=pki_[:, k:k + 1], axis=0),
                        in_=hb_[:], in_offset=None), [t_pki, t_hb], [])
                yield

            run_interleaved((tile6(ti) for ti in range(NT)), 3)

        with (Phase(nc, "p7", G) if stop >= 7 else Skip()) as P:
            w1b = [(w1b0, [Tok() for _ in range(8)]), P.sb([128, 8, 2 * D], BF16, "w1b", ntok=8)]
            w2b = [(w2b0, [Tok() for _ in range(8)]), P.sb([128, 8, D], BF16, "w2b", ntok=8)]
            b1t = [(b1t0, Tok()), P.sb([128, 16], F32, "b1t")]
            b2t = [(b2t0, Tok()), P.sb([128, D], F32, "b2t")]
            xr = [P.sb([128, CT, D], BF16, "xr") for _ in range(2)]
            xTb = [P.sb([128, 8, CAP], BF16, "xT") for _ in range(2)]
            NBUF = 3
            gtb = [P.sb([128, NH], F32, "gtb") for _ in range(NBUF)]
            sgb = [P.sb([128, NH], F32, "sgb") for _ in range(NBUF)]
            ubb = [P.sb([128, NH], F32, "ubb") for _ in range(NBUF)]
            aT = [P.sb([128, 8, NH], BF16, "aT") for _ in range(2)]
            yo = [P.sb([128, D], BF16, "yo") for _ in range(3)]
            ptr = [P.ps([128, 8, 128], BF16, "ptr") for _ in range(2)]
            pg = [P.ps([128, NH], F32, "pg") for _ in range(2)]
            pu = [P.ps([128, NH], F32, "pu") for _ in range(2)]
            py = [P.ps([128, 512], F32, "py") for _ in range(2)]
            st7 = {"gi": 0, "yi": 0, "pyi": 0}

            def load(ex, weights=True):
                (w1_, t_w1), (w2_, t_w2), (b1_, t_b1), (b2_, t_b2), (xr_, t_xr) = \
                    w1b[ex % 2], w2b[ex % 2], b1t[ex % 2], b2t[ex % 2], xr[ex % 2]
                P.dma("sp", xr_[:], xs_d[ex * CAP:(ex + 1) * CAP, :].rearrange("(t p) d -> p t d", p=128), writes=[t_xr])
                if not weights:
                    return
                for kc in range(8):
                    P.dma("pool", w1_[:, kc, :], w1[ex, kc * 128:(kc + 1) * 128, :], writes=[t_w1[kc]])
                for kc in range(8):
                    P.dma("pool", w2_[:, kc, :], w2[ex, kc * 128:(kc + 1) * 128, :], writes=[t_w2[kc]])
                P.dma("sp", b1_[:], b1[ex], writes=[t_b1])
                P.dma("sp", b2_[:], b2[ex].partition_broadcast(128), writes=[t_b2])

            def transposes(ex):
                xr_, t_xr = xr[ex % 2]
                xT, t_xT = xTb[ex % 2]
                for t in range(CT):
                    pt, t_pt = ptr[t % 2]
                    for kc in range(8):
                        P.tr(pt[:, kc, :], xr_[:, t, kc * 128:(kc + 1) * 128], ident_b[:], reads=[t_xr], writes=[t_pt])
                    P.copy("act" if t % 2 else "dve", xT[:, :, t * 128:(t + 1) * 128], pt[:], reads=[t_pt], writes=[t_xT])

            def pair_front(ex, sh, j):
                (w1_, t_w1), (b1_, t_b1) = w1b[ex % 2], b1t[ex % 2]
                xT, t_xT = xTb[ex % 2]
                a_, t_a = aT[(ex * NHALF + sh) % 2]
                ssl = slice(sh * NH, (sh + 1) * NH)
                gi = st7["gi"]
                st7["gi"] += 1
                (pg_, t_pg), (pu_, t_pu) = pg[gi % 2], pu[gi % 2]
                (g_, t_g), (s_, t_s), (u_, t_u) = gtb[gi % NBUF], sgb[gi % NBUF], ubb[gi % NBUF]
                for kc in range(8):
                    P.mm(pg_[:, 0:NH], w1_[:, kc, j * 128:(j + 1) * 128], xT[:, kc, ssl], kc == 0, kc == 7,
                         reads=[t_w1[kc], t_xT], writes=[t_pg])
                for kc in range(8):
                    P.mm(pu_[:, 0:NH], w1_[:, kc, D + j * 128:D + (j + 1) * 128], xT[:, kc, ssl], kc == 0, kc == 7,
                         reads=[t_w1[kc], t_xT], writes=[t_pu])
                P.ts("dve", g_[:], pg_[:, 0:NH], b1_[:, j:j + 1], 7.0, ALU.add, ALU.min, reads=[t_pg, t_b1], writes=[t_g])
                P.act(u_[:], pu_[:, 0:NH], AF.Identity, reads=[t_pu, t_b1], writes=[t_u], bias=b1_[:, 8 + j:9 + j])
                P.act(s_[:], g_[:], AF.Sigmoid, reads=[t_g], writes=[t_s], scale=1.702)
                P.ts("pool", u_[:], u_[:], 7.0, -7.0, ALU.min, ALU.max, reads=[t_u], writes=[t_u])
                P.tt("pool", s_[:], g_[:], s_[:], ALU.mult, reads=[t_g, t_s], writes=[t_s])

                def back():
                    P.stt(a_[:, j, :], u_[:], 1.0, s_[:], ALU.add, ALU.mult, reads=[t_u, t_s], writes=[t_a])
                return back

            def ymm(ex, sh):
                (w2_, t_w2), (b2_, t_b2) = w2b[ex % 2], b2t[ex % 2]
                a_, t_a = aT[(ex * NHALF + sh) % 2]
                for t in range(NH // 128):
                    row0 = ex * CAP + sh * NH + t * 128
                    y_, t_y = yo[st7["yi"] % 3]
                    st7["yi"] += 1
                    for nh in range(2):
                        py_, t_py = py[st7["pyi"] % 2]
                        st7["pyi"] += 1
                        for j in range(8):
                            P.mm(py_[:], a_[:, j, t * 128:(t + 1) * 128], w2_[:, j, nh * 512:(nh + 1) * 512], j == 0, j == 7,
                                 reads=[t_a, t_w2[j]], writes=[t_py])
                        P.tt("dve", y_[:, nh * 512:(nh + 1) * 512], py_[:], b2_[:, nh * 512:(nh + 1) * 512], ALU.add,
                             reads=[t_py, t_b2], writes=[t_y])
                    P.dma("sp", ys_d[row0:row0 + 128, :], y_[:], reads=[t_y])

            load(0, weights=False)
            transposes(0)
            prev_unit = None
            for ex in range(NE):
                for sh in range(NHALF):
                    pend = None
                    for j in range(8):
                        back = pair_front(ex, sh, j)
                        if pend is not None:
                            pend()
                        pend = back
                        if j == 1 and prev_unit is not None:
                            ymm(*prev_unit)
                            prev_unit = None
                        if j == 1 and sh == 0 and ex + 1 < NE:
                            load(ex + 1)
                        if j == 4 and sh == NHALF - 1 and ex + 1 < NE:
                            transposes(ex + 1)
                    pend()
                    if prev_unit is not None:
                        ymm(*prev_unit)
                    prev_unit = (ex, sh)
            ymm(*prev_unit)

        pre7.close()

        with (Phase(nc, "p8", G) if stop >= 8 else Skip()) as P:
            P.eps_ap = eps_t[:]
            gfin, t_gfin = P.sb([128, D], F32, "gfin")
            P.dma("sp", gfin[:], g_final.partition_broadcast(128), writes=[t_gfin])
            NB8 = 7
            junks = [P.sb([128, D], BF16, "junk") for _ in range(NB8)]
            xt = [P.sb([128, D], F32, "x") for _ in range(NB8)]
            yk = [[P.sb([128, D], BF16, "yk") for _ in range(4)] for _ in range(NB8)]
            pki = [P.sb([128, 4], I32, "pki") for _ in range(NB8)]
            gt = [P.sb([128, 4], F32, "gt") for _ in range(NB8)]
            sq = [P.sb([128, 1], F32, "sq") for _ in range(NB8)]
            ot = [P.sb([128, D], F32, "o") for _ in range(NB8)]

            def tile8(ti):
                tok0 = ti * 128
                b = ti % NB8
                (x_, t_x), (pki_, t_pki), (g_, t_g), (s_, t_s), (o_, t_o) = xt[b], pki[b], gt[b], sq[b], ot[b]
                junk, t_junk = junks[b]
                P.dma("act", pki_[:], pos_d[tok0:tok0 + 128, :], writes=[t_pki])
                P.dma("act", x_[:], x2_d[tok0:tok0 + 128, :], writes=[t_x])
                P.dma("act", g_[:], gate_d[tok0:tok0 + 128, :], writes=[t_g])
                yield
                for k in range(4):
                    y_, t_y = yk[b][k]
                    P.S.dma("pool", lambda e, pki_=pki_, y_=y_, k=k: e.indirect_dma_start(
                        out=y_[:], out_offset=None, in_=ys_d,
                        in_offset=bass.IndirectOffsetOnAxis(ap=pki_[:, k:k + 1], axis=0)), [t_pki], [t_y])
                yield
                for k in range(4):
                    y_, t_y = yk[b][k]
                    P.stt(x_[:], y_[:], g_[:, k:k + 1], x_[:], ALU.mult, ALU.add, reads=[t_y, t_g, t_x], writes=[t_x])
                    yield
                P.act(junk[:], x_[:], AF.Square, reads=[t_x], writes=[t_junk, t_s], accum_out=s_[:])
                yield
                P.act(s_[:], s_[:], AF.Ln, reads=[t_s], writes=[t_s], scale=1.0 / D, bias=eps_t[:])
                yield
                P.act(s_[:], s_[:], AF.Exp, reads=[t_s], writes=[t_s], scale=-0.5)
                yield
                P.stt(o_[:], x_[:], s_[:, 0:1], gfin[:], ALU.mult, ALU.mult, reads=[t_x, t_s, t_gfin], writes=[t_o])
                yield
                P.dma("sp", out[tok0:tok0 + 128, :], o_[:], reads=[t_o])
                yield

            run_interleaved((tile8(ti) for ti in range(NT)), 6)
    return nc


def make_consts(CAP):
    j = np.arange(128)[:, None]
    s = np.arange(128)[None, :]
    c = {}
    c["c_ident"] = np.eye(128, dtype=np.float32)
    c["c_tri1"] = np.where(j >= s, -1.0, 0.0).astype(np.float32)
    c["c_tri2"] = np.where(j < s, -1.0, 0.0).astype(np.float32)
    c["c_ltri"] = np.where(j < s, 1.0, 0.0).astype(np.float32)
    c["c_ones"] = np.ones((128, 128), np.float32)
    m = np.zeros((4, 128, 512), np.float32)
    for jj in range(4):
        for cb in range(4):
            if cb > jj:
                m[jj, :, cb * 128:(cb + 1) * 128] = 1.0
            elif cb == jj:
                m[jj, :, cb * 128:(cb + 1) * 128] = (j < s).astype(np.float32)
    c["c_mask"] = m
    c["c_eoff"] = np.broadcast_to((np.arange(NE) * CAP).astype(np.float32)[None, :], (128, NE)).copy()
    c["c_rcnt"] = np.broadcast_to((1.0 / (np.arange(16) + 1)).astype(np.float32)[None, :], (128, 16)).copy()
    return c


def make_in_maps(inputs, n_cores, SEQ, CAP):
    f = lambda a: np.ascontiguousarray(np.asarray(a, dtype=np.float32))
    I = {k: np.asarray(v) for k, v in inputs.items()}
    shared = dict(make_consts(CAP))
    shared["g_mix"] = f(I["g_mix"][0][None, :])
    shared["w_in"] = f(I["w_in"][0])
    shared["g_sb"] = f(I["g_sb_out"][0].reshape(4, 128).T)
    shared["g_pool"] = f(I["g_pool_out"][0].reshape(4, 128).T)
    shared["w_pool"] = f(I["w_pool"][0])
    shared["pool_scale"] = f(I["pool_scale"][0].reshape(4, 128).T)
    shared["w_out"] = f(I["w_out"][0])
    shared["g_mem_q"] = f(I["g_mem_q"][0][None, :])
    shared["g_mem_kv"] = f(I["g_mem_kv"][0][None, :])
    shared["w_mem_q"] = f(I["w_mem_q"][0])
    shared["w_mem_kv"] = f(I["w_mem_kv"][0])
    shared["w_mem_o"] = f(I["w_mem_o"][0])
    shared["g_ffn"] = f(I["g_ffn"][0][None, :])
    shared["w_router"] = f(I["w_router"][0])
    shared["b_router"] = f(I["b_router"][0][None, :])
    shared["w1"] = f(I["w_expert_in"][0])
    shared["b1"] = f(I["b_expert_in"][0].reshape(NE, 16, 128).transpose(0, 2, 1))
    shared["w2"] = f(I["w_expert_out"][0])
    shared["b2"] = f(I["b_expert_out"][0][:, None, :])
    shared["g_final"] = f(I["g_final"][None, :])
    maps = []
    for c in range(n_cores):
        m = dict(shared)
        m["x"] = f(I["x"][c, :SEQ])
        m["mem"] = f(I["mem"][c])
        maps.append(m)
    return maps


def kernel(**inputs):
    SEQ, CAP = 4096, 768
    n = 8
    nc = build(SEQ, CAP)
    maps = make_in_maps(inputs, n, SEQ, CAP)
    res = run_bass_kernel_spmd(nc, maps, core_ids=list(range(n)))
    return np.stack([np.asarray(r["out"], dtype=np.float32) for r in res.results], axis=0)
```

```python
import contextlib
import numpy as np
import concourse.bass as bass
import concourse.mybir as mybir
from concourse.bass_utils import run_bass_kernel_spmd

F32 = mybir.dt.float32
BF16 = mybir.dt.bfloat16
I32 = mybir.dt.int32
AF = mybir.ActivationFunctionType
ALU = mybir.AluOpType

N_DMA_SEMS = 32
D = 1024
NE = 32
NMEM = 256
EPS = 1e-5


class Tok:
    __slots__ = ("last_w", "readers")

    def __init__(self):
        self.last_w = None
        self.readers = []


class Op:
    __slots__ = ("eng", "fn", "deps", "signaled", "sem", "val", "is_dma")

    def __init__(self, eng, fn, is_dma):
        self.eng = eng
        self.fn = fn
        self.deps = []
        self.signaled = False
        self.sem = None
        self.val = 0
        self.is_dma = is_dma


class Sched:
    ENGS = ("pe", "act", "dve", "pool", "sp")

    def __init__(self, G):
        self.G = G
        self.ops = {e: [] for e in self.ENGS}
        self.dma_rr = {"sp": 0, "pool": 0, "act": 0}
        self.dma_last = [None] * N_DMA_SEMS
        self.dma_cnt = G["dma_cnt"]

    def _add(self, eng, fn, reads, writes, is_dma):
        op = Op(eng, fn, is_dma)
        deps = []
        for t in reads:
            if t.last_w is not None:
                deps.append(t.last_w)
        for t in writes:
            if t.last_w is not None:
                deps.append(t.last_w)
            deps.extend(t.readers)
        if is_dma:
            half = N_DMA_SEMS // 2
            j = self.dma_rr[eng] + (half if eng == "pool" else 0)
            self.dma_rr[eng] = (self.dma_rr[eng] + 1) % half
            if self.dma_last[j] is not None:
                deps.append(self.dma_last[j])
            self.dma_last[j] = op
            self.dma_cnt[j] += 16
            op.sem = ("dma", j)
            op.val = self.dma_cnt[j]
            op.signaled = True
        seen = set()
        for d in deps:
            if id(d) in seen:
                continue
            seen.add(id(d))
            if (not d.is_dma) and (not is_dma) and d.eng == eng and eng == "pe":
                continue
            op.deps.append(d)
            if not d.is_dma:
                d.signaled = True
        for t in reads:
            t.readers.append(op)
        for t in writes:
            t.last_w = op
            t.readers = []
        self.ops[eng].append(op)
        return op

    def op(self, eng, fn, reads=(), writes=()):
        return self._add(eng, fn, reads, writes, False)

    def dma(self, eng, fn, reads=(), writes=()):
        return self._add(eng, fn, reads, writes, True)

    def emit(self, nc, tag):
        for e in self.ENGS:
            for op in reversed(self.ops[e]):
                if not op.is_dma:
                    op.signaled = True
                    break
        for e in self.ENGS:
            c = self.G["eng_cnt"][e]
            for op in self.ops[e]:
                if op.is_dma:
                    continue
                if op.signaled:
                    c += 1
                    op.sem = ("eng", e)
                    op.val = c
            self.G["eng_cnt"][e] = c
        with contextlib.ExitStack() as st:
            sems = self.G["sems"]
            block = st.enter_context(nc.Block())
            lasts = []
            for j in range(N_DMA_SEMS):
                if self.dma_last[j] is not None:
                    lasts.append(self.dma_last[j])
            for e in self.ENGS:
                for op in reversed(self.ops[e]):
                    if not op.is_dma:
                        lasts.append(op)
                        break
            for e in self.ENGS:
                fin = Op(e, None, False)
                fin.deps = list(lasts)
                self.ops[e].append(fin)

            def run(engname, eng):
                waited = {}
                for op in self.ops[engname]:
                    need = {}
                    for d in op.deps:
                        if need.get(d.sem, 0) < d.val:
                            need[d.sem] = d.val
                    for s, v in need.items():
                        if waited.get(s, 0) < v:
                            eng.wait_ge(sems[s], v)
                            waited[s] = v
                    if op.fn is None:
                        continue
                    ins = op.fn(eng)
                    if op.is_dma:
                        ins.then_inc(sems[op.sem], 16)
                    elif op.signaled:
                        ins.then_inc(sems[op.sem], 1)

            block.tensor(lambda eng: run("pe", eng))
            block.scalar(lambda eng: run("act", eng))
            block.vector(lambda eng: run("dve", eng))
            block.gpsimd(lambda eng: run("pool", eng))
            block.sync(lambda eng: run("sp", eng))


class _Any:
    def __getattr__(self, k):
        return self

    def __call__(self, *a, **k):
        return self

    def __getitem__(self, k):
        return self

    def __iter__(self):
        return iter((self, self))


class Skip:
    def __enter__(self):
        return _Any()

    def __exit__(self, *a):
        return False


def run_interleaved(gens, width):
    active = []
    it = iter(gens)
    done = False
    while True:
        while not done and len(active) < width:
            g = next(it, None)
            if g is None:
                done = True
                break
            active.append(g)
        if not active:
            break
        for g in list(active):
            try:
                next(g)
            except StopIteration:
                active.remove(g)


class Phase:
    def __init__(self, nc, tag, G):
        self.nc = nc
        self.tag = tag
        self.S = Sched(G)
        self.st = contextlib.ExitStack()
        self.n = 0

    def __enter__(self):
        self.st.__enter__()
        return self

    def __exit__(self, *a):
        if a[0] is None:
            self.S.emit(self.nc, self.tag)
        return self.st.__exit__(*a)

    def sb(self, shape, dt, name=None, ntok=None):
        self.n += 1
        t = self.st.enter_context(self.nc.sbuf_tensor("%s_%s%d" % (self.tag, name or "t", self.n), list(shape), dt))
        if ntok is not None:
            return t, [Tok() for _ in range(ntok)]
        return t, Tok()

    def ps(self, shape, dt, name=None):
        self.n += 1
        esz = 2 if dt == BF16 else 4
        free = 1
        for d_ in shape[1:]:
            free *= d_
        nbytes = free * esz
        assert nbytes % 2048 == 0 or len(shape) == 2, shape
        shape = list(shape)
        if nbytes % 2048 != 0:
            shape[1] = ((nbytes + 2047) // 2048) * 2048 // esz
        shape[0] = 128
        t = self.st.enter_context(self.nc.psum_tensor("%s_%s%d" % (self.tag, name or "p", self.n), shape, dt))
        return t, Tok()

    def dma(self, q, out, in_, reads=(), writes=()):
        return self.S.dma(q, lambda e: e.dma_start(out=out, in_=in_), reads, writes)

    def mm(self, out, lhsT, rhs, start, stop, reads=(), writes=()):
        return self.S.op("pe", lambda e: e.matmul(out, lhsT, rhs, start=start, stop=stop), reads, writes)

    def tr(self, out, in_, ident, reads=(), writes=()):
        return self.S.op("pe", lambda e: e.transpose(out, in_, ident), reads, writes)

    def act(self, out, in_, func, reads=(), writes=(), **kw):
        return self.S.op("act", lambda e: e.activation(out, in_, func, **kw), reads, writes)

    def copy(self, eng, out, in_, reads=(), writes=()):
        if eng == "act":
            return self.S.op("act", lambda e: e.copy(out, in_), reads, writes)
        return self.S.op(eng, lambda e: e.tensor_copy(out, in_), reads, writes)

    def tt(self, eng, out, a, b, op, reads=(), writes=()):
        return self.S.op(eng, lambda e: e.tensor_tensor(out=out, in0=a, in1=b, op=op), reads, writes)

    def ts(self, eng, out, a, s1, s2, op0, op1=None, reads=(), writes=(), accum_out=None):
        if op1 is None:
            return self.S.op(eng, lambda e: e.tensor_scalar(out=out, in0=a, scalar1=s1, scalar2=None, op0=op0), reads, writes)
        if accum_out is not None:
            return self.S.op(eng, lambda e: e.tensor_scalar(out=out, in0=a, scalar1=s1, scalar2=s2, op0=op0, op1=op1,
                                                            accum_out=accum_out), reads, writes)
        return self.S.op(eng, lambda e: e.tensor_scalar(out=out, in0=a, scalar1=s1, scalar2=s2, op0=op0, op1=op1), reads, writes)

    def stt(self, out, in0, scalar, in1, op0, op1, reads=(), writes=(), accum_out=None):
        if accum_out is not None:
            return self.S.op("dve", lambda e: e.scalar_tensor_tensor(out=out, in0=in0, scalar=scalar, in1=in1, op0=op0,
                                                                     op1=op1, accum_out=accum_out), reads, writes)
        return self.S.op("dve", lambda e: e.scalar_tensor_tensor(out=out, in0=in0, scalar=scalar, in1=in1, op0=op0, op1=op1),
                         reads, writes)

    def rstd(self, rs, ss, n, t_ss, t_rs):
        self.act(rs, ss, AF.Ln, reads=[t_ss], writes=[t_rs], scale=1.0 / n, bias=self.eps_ap)
        self.act(rs, rs, AF.Exp, reads=[t_rs], writes=[t_rs], scale=-0.5)


def build(SEQ, CAP, dbg=False, stop=99):
    NT = SEQ // 128
    NCH = SEQ // 512
    NSLOT = NE * CAP
    TRASH = NSLOT
    CT = CAP // 128
    NH = CAP // 2
    assert NH <= 512 and CAP % 256 == 0 or CT == 1
    if CT == 1:
        NH = CAP
    NHALF = CAP // NH

    nc = bass.Bass("TRN2", target_bir_lowering=False)

    def din(name, shape, dt=F32):
        return nc.dram_tensor(name, list(shape), dt, kind="ExternalInput").ap()

    def dscr(name, shape, dt=F32):
        return nc.dram_tensor(name, list(shape), dt, kind="ExternalOutput" if dbg else "Internal").ap()

    x = din("x", [SEQ, D])
    mem = din("mem", [NMEM, D])
    g_mix = din("g_mix", [1, D])
    w_in = din("w_in", [D, 2048])
    g_sb = din("g_sb", [128, 4])
    g_pool = din("g_pool", [128, 4])
    w_pool = din("w_pool", [4, 128, 128])
    pool_scale = din("pool_scale", [128, 4])
    w_out = din("w_out", [D, D])
    g_mem_q = din("g_mem_q", [1, D])
    g_mem_kv = din("g_mem_kv", [1, D])
    w_mem_q = din("w_mem_q", [D, D])
    w_mem_kv = din("w_mem_kv", [D, 2 * D])
    w_mem_o = din("w_mem_o", [D, D])
    g_ffn = din("g_ffn", [1, D])
    w_router = din("w_router", [D, NE])
    b_router = din("b_router", [1, NE])
    w1 = din("w1", [NE, D, 2 * D])
    b1 = din("b1", [NE, 128, 16])
    w2 = din("w2", [NE, D, D])
    b2 = din("b2", [NE, 1, D])
    g_final = din("g_final", [1, D])
    c_ident = din("c_ident", [128, 128])
    c_tri1 = din("c_tri1", [128, 128])
    c_tri2 = din("c_tri2", [128, 128])
    c_ltri = din("c_ltri", [128, 128])
    c_ones = din("c_ones", [128, 128])
    c_mask = din("c_mask", [4, 128, 512])
    c_eoff = din("c_eoff", [128, NE])
    c_rcnt = din("c_rcnt", [128, 16])
    out = nc.dram_tensor("out", [SEQ, D], F32, kind="ExternalOutput").ap()

    uT_d = dscr("uT_d", [512, SEQ])
    sbT_d = dscr("sbT_d", [512, SEQ])
    yT_d = dscr("yT_d", [512, SEQ])
    x1_d = dscr("x1_d", [SEQ, D])
    x2_d = dscr("x2_d", [SEQ, D])
    xs_d = dscr("xs_d", [NSLOT + 128, D], BF16)
    ys_d = dscr("ys_d", [NSLOT + 128, D], BF16)
    pos_d = dscr("pos_d", [SEQ, 4], I32)
    gate_d = dscr("gate_d", [SEQ, 4])
    t_uT, t_sbT, t_yT, t_x1, t_x2, t_xs, t_ys, t_pos, t_gate = [Tok() for _ in range(9)]

    with contextlib.ExitStack() as top:
        def gsb(name, shape, dt):
            return top.enter_context(nc.sbuf_tensor(name, list(shape), dt))
        G = {"sems": {}, "eng_cnt": {e: 0 for e in Sched.ENGS}, "dma_cnt": [0] * N_DMA_SEMS}
        for e in Sched.ENGS:
            G["sems"][("eng", e)] = top.enter_context(nc.semaphore("s_" + e))
        for j in range(N_DMA_SEMS):
            G["sems"][("dma", j)] = top.enter_context(nc.semaphore("s_d%d" % j))
        eps_t = gsb("eps_t", [128, 1], F32)
        ident_f = gsb("ident_f", [128, 128], F32)
        ident_b = gsb("ident_b", [128, 128], BF16)
        ones_b = gsb("ones_b", [128, 128], BF16)
        KT = gsb("KT", [128, 8, NMEM], BF16)
        V = gsb("V", [128, 2, D], BF16)
        qkv = contextlib.ExitStack()
        qT = qkv.enter_context(nc.sbuf_tensor("qT", [128, 4, SEQ], BF16))
        kT = qkv.enter_context(nc.sbuf_tensor("kT", [128, 4, SEQ], BF16))
        vv = qkv.enter_context(nc.sbuf_tensor("vv", [128, NT, 512], BF16))

        with Phase(nc, "p0", G) as P:
            P.S.op("dve", lambda e: e.memset(eps_t[:], EPS))
            P.dma("sp", ident_f[:], c_ident)
            P.dma("pool", ident_b[:], c_ident)
            P.dma("pool", ones_b[:], c_ones)

        with (Phase(nc, "p1", G) if stop >= 1 else Skip()) as P:
            P.eps_ap = eps_t[:]
            w_bf, t_wl = P.sb([128, 8, 2048], BF16, "win", ntok=8)
            gbc, t_gbc = P.sb([128, D], F32, "gbc")
            for kc in range(8):
                P.dma("pool", w_bf[:, kc, :], w_in[kc * 128:(kc + 1) * 128, :], writes=[t_wl[kc]])
            P.dma("sp", gbc[:], g_mix.partition_broadcast(128), writes=[t_gbc])
            xt = [P.sb([128, D], F32, "x") for _ in range(3)]
            junks = [P.sb([128, D], BF16, "junk") for _ in range(2)]
            hb = [P.sb([128, D], BF16, "h") for _ in range(8)]
            ss = [P.sb([128, 1], F32, "ss") for _ in range(4)]
            hT = [P.sb([128, 8, 512], BF16, "hT") for _ in range(2)]
            ust = [P.sb([128, 512], F32, "ust") for _ in range(2)]
            ptr = [P.ps([128, 8, 128], BF16, "ptr") for _ in range(2)]
            pmm = [P.ps([128, 512], F32, "pmm") for _ in range(4)]
            st1 = {"mi": 0}

            def tileA1(c, t):
                ti = c * 4 + t
                tok0 = ti * 128
                xs_, t_x = xt[ti % 3]
                h_, t_h = hb[ti % 8]
                s_, t_s = ss[ti % 4]
                junk, t_junk = junks[ti % 2]
                P.dma("sp", xs_[:], x[tok0:tok0 + 128, :], writes=[t_x])
                yield
                P.act(junk[:], xs_[:], AF.Square, reads=[t_x], writes=[t_junk, t_s], accum_out=s_[:])
                yield
                P.act(s_[:], s_[:], AF.Ln, reads=[t_s], writes=[t_s], scale=1.0 / D, bias=eps_t[:])
                yield
                P.act(s_[:], s_[:], AF.Exp, reads=[t_s], writes=[t_s], scale=-0.5)
                yield
                P.stt(h_[:], xs_[:], s_[:, 0:1], gbc[:], ALU.mult, ALU.mult, reads=[t_x, t_s, t_gbc], writes=[t_h])
                yield

            def prepB1(c):
                hTc, t_hT = hT[c % 2]
                for t in range(4):
                    ti = c * 4 + t
                    h_, t_h = hb[ti % 8]
                    pt, t_pt = ptr[ti % 2]
                    for kc in range(8):
                        P.tr(pt[:, kc, :], h_[:, kc * 128:(kc + 1) * 128], ident_b[:], reads=[t_h], writes=[t_pt])
                    P.copy("dve" if t % 2 else "act", hTc[:, :, t * 128:(t + 1) * 128], pt[:], reads=[t_pt], writes=[t_hT])

            def mm1(c):
                hTc, t_hT = hT[c % 2]
                for j in range(12):
                    col = j * 128 if j < 8 else 1536 + (j - 8) * 128
                    pm, t_pm = pmm[st1["mi"] % 4]
                    st1["mi"] += 1
                    for kc in range(8):
                        P.mm(pm[:], w_bf[:, kc, col:col + 128], hTc[:, kc, :], kc == 0, kc == 7,
                             reads=[t_wl[kc], t_hT], writes=[t_pm])
                    if j < 4:
                        P.copy("act", qT[:, j, c * 512:(c + 1) * 512], pm[:], reads=[t_pm], writes=[])
                    elif j < 8:
                        P.copy("dve", kT[:, j - 4, c * 512:(c + 1) * 512], pm[:], reads=[t_pm], writes=[])
                    else:
                        us, t_us = ust[j % 2]
                        P.copy("act" if j % 2 else "dve", us[:], pm[:], reads=[t_pm], writes=[t_us])
                        P.dma("sp", uT_d[(j - 8) * 128:(j - 7) * 128, c * 512:(c + 1) * 512], us[:], reads=[t_us])
                for t in range(4):
                    pm, t_pm = pmm[st1["mi"] % 4]
                    st1["mi"] += 1
                    for kc in range(8):
                        P.mm(pm[:], hTc[:, kc, t * 128:(t + 1) * 128], w_bf[:, kc, 1024:1536], kc == 0, kc == 7,
                             reads=[t_wl[kc], t_hT], writes=[t_pm])
                    P.copy("act" if t % 2 else "dve", vv[:, c * 4 + t, :], pm[:], reads=[t_pm], writes=[])

            run_interleaved((tileA1(0, t) for t in range(4)), 2)
            prepB1(0)
            for c in range(NCH):
                if c + 1 < NCH:
                    run_interleaved((tileA1(c + 1, t) for t in range(4)), 2)
                mm1(c)
                if c + 1 < NCH:
                    prepB1(c + 1)

        with (Phase(nc, "p2", G) if stop >= 2 else Skip()) as P:
            tri1, t_c = P.sb([128, 128], BF16, "tri1")
            tri2, _ = P.sb([128, 128], BF16, "tri2")
            mask, _ = P.sb([128, 4, 2, 512], BF16, "mask")
            P.dma("pool", tri1[:], c_tri1, writes=[t_c])
            P.dma("pool", tri2[:], c_tri2, writes=[t_c])
            for j in range(4):
                for s_ in range(2):
                    P.dma("pool", mask[:, j, s_, :], c_mask[j], writes=[t_c])
            NB = 3
            Eb = [P.sb([128, 2, 512], F32, "E") for _ in range(NB)]
            Lb = [P.sb([128, 2, 512], BF16, "L") for _ in range(NB)]
            Xb = [P.sb([128, 2, 512], F32, "X") for _ in range(NB)]
            Wb = [P.sb([128, 2, 512], BF16, "W") for _ in range(NB)]
            Ob = [P.sb([64, 512], F32, "O") for _ in range(2)]
            Zp = [P.ps([128, 2, 512], F32, "Z") for _ in range(2)]
            Gp, t_g = P.ps([128, 2, 512], F32, "G")
            Op_ = [P.ps([64, 512], F32, "Oa") for _ in range(2)]
            class Seg:
                def __init__(self, hp, qc, n0):
                    self.hp, self.qc, self.n0 = hp, qc, n0
                    self.nb = 4 * qc + 4
                    self.q0 = qc * 512

                def diag(self, i):
                    return (self.nb - 1 - i) >= 4 * self.qc

                def cs(self, i):
                    j = self.nb - 1 - i - 4 * self.qc
                    return slice(128 * j, 512) if j > 0 else slice(0, 512)

                def qk(self, i):
                    kb = self.nb - 1 - i
                    z, t_z = Zp[(self.n0 + i) % 2]
                    c = self.cs(i)
                    for s in range(2):
                        pb = s * 64
                        P.mm(z[:, s, c], kT[pb:pb + 64, self.hp, kb * 128:(kb + 1) * 128],
                             qT[pb:pb + 64, self.hp, self.q0 + c.start:self.q0 + 512], True, True, writes=[t_z])

                def front(self, i):
                    (z, t_z), (E, t_E) = Zp[(self.n0 + i) % 2], Eb[(self.n0 + i) % NB]
                    c = self.cs(i)
                    P.act(E[:, :, c], z[:, :, c], AF.Exp, reads=[t_z], writes=[t_E], scale=0.125)

                def midA(self, i):
                    k = (self.n0 + i) % NB
                    (E, t_E), (L, t_L) = Eb[k], Lb[k]
                    c = self.cs(i)
                    P.act(L[:, :, c], E[:, :, c], AF.Ln, reads=[t_E], writes=[t_L], bias=1.0)
                    if self.diag(i):
                        j = self.nb - 1 - i - 4 * self.qc
                        P.tt("dve", L[:, :, c], L[:, :, c], mask[:, j, :, c], ALU.mult, reads=[t_L, t_c], writes=[t_L])

                def midB(self, i):
                    L, t_L = Lb[(self.n0 + i) % NB]
                    c = self.cs(i)
                    for s in range(2):
                        P.mm(Gp[:, s, c], tri1[:], L[:, s, c], i == 0, False, reads=[t_L, t_c], writes=[t_g])

                def backA(self, i):
                    k = (self.n0 + i) % NB
                    (L, t_L), (X, t_X) = Lb[k], Xb[k]
                    c = self.cs(i)
                    P.act(X[:, :, c], Gp[:, :, c], AF.Exp, reads=[t_g], writes=[t_X])
                    if i + 1 < self.nb:
                        for s in range(2):
                            P.mm(Gp[:, s, c], tri2[:], L[:, s, c], False, False, reads=[t_L, t_c], writes=[t_g])

                def backB(self, i):
                    k = (self.n0 + i) % NB
                    (E, t_E), (X, t_X), (W, t_W) = Eb[k], Xb[k], Wb[k]
                    c = self.cs(i)
                    P.tt("dve", W[:, :, c], E[:, :, c], X[:, :, c], ALU.mult, reads=[t_E, t_X], writes=[t_W])
                    if self.diag(i):
                        j = self.nb - 1 - i - 4 * self.qc
                        P.tt("dve", W[:, :, c], W[:, :, c], mask[:, j, :, c], ALU.mult, reads=[t_W, t_c], writes=[t_W])

                def pv(self, i):
                    kb = self.nb - 1 - i
                    W, t_W = Wb[(self.n0 + i) % NB]
                    c = self.cs(i)
                    for s in range(2):
                        o, t_o = Op_[s]
                        h = self.hp * 2 + s
                        P.mm(o[0:64, c], vv[:, kb, h * 64:(h + 1) * 64], W[:, s, c], i == 0, i == self.nb - 1,
                             reads=[t_W], writes=[t_o])

                def tail(self):
                    last = self.nb - 1
                    self.backB(last)
                    self.pv(last)
                    for s in range(2):
                        o, t_o = Op_[s]
                        ob, t_ob = Ob[s]
                        h = self.hp * 2 + s
                        P.copy("dve", ob[:], o[0:64, :], reads=[t_o], writes=[t_ob])
                        P.dma("sp", sbT_d[h * 64:(h + 1) * 64, self.q0:self.q0 + 512], ob[:], reads=[t_ob])

            segs = []
            n0 = 0
            for hp in range(4):
                for qc in range(NCH):
                    segs.append(Seg(hp, qc, n0))
                    n0 += segs[-1].nb
            segs[0].qk(0)
            prev = None
            for k, F in enumerate(segs):
                for i in range(F.nb):
                    F.front(i)
                    if i + 1 < F.nb:
                        F.qk(i + 1)
                    elif k + 1 < len(segs):
                        segs[k + 1].qk(0)
                    dg = F.diag(i)
                    if dg:
                        F.midA(i)
                    if i > 0:
                        F.backA(i - 1)
                    elif prev is not None:
                        prev.backA(prev.nb - 1)
                    if not dg:
                        F.midA(i)
                    F.midB(i)
                    if i > 0:
                        F.backB(i - 1)
                        F.pv(i - 1)
                    elif prev is not None:
                        prev.tail()
                prev = F
            prev.backA(prev.nb - 1)
            prev.tail()

        qkv.close()

        with (Phase(nc, "p3", G) if stop >= 3 else Skip()) as P:
            wp, t_wp = P.sb([128, 4, 128], BF16, "wp")
            psc, t_psc = P.sb([128, 4], F32, "psc")
            rc, t_rc = P.sb([128, 16], F32, "rc")
            for g in range(4):
                P.dma("pool", wp[:, g, :], w_pool[g], writes=[t_wp])
            P.dma("sp", psc[:], pool_scale, writes=[t_psc])
            P.dma("sp", rc[:], c_rcnt, writes=[t_rc])
            ub = [P.sb([128, SEQ], F32, "u") for _ in range(4)]
            wa2 = [[P.sb([128, SEQ], F32, "wa") for _ in range(2)] for _ in range(2)]
            pl = [P.sb([128, SEQ], BF16, "pl") for _ in range(2)]
            yst = [P.sb([128, 512], F32, "y") for _ in range(2)]
            pp = [P.ps([128, 512], F32, "pp") for _ in range(2)]
            k = 0
            for g in range(4):
                W = 2 << g
                u, t_u = ub[g]
                P.dma("act", u[:], uT_d[g * 128:(g + 1) * 128, :], writes=[t_u])
                eng = "dve" if g % 2 == 0 else "pool"
                wa = wa2[g % 2]
                src, t_src = u, t_u
                sh = 1
                step = 0
                while sh < W:
                    dst, t_dst = wa[step % 2]
                    P.tt(eng, dst[:, sh:SEQ], src[:, sh:SEQ], src[:, 0:SEQ - sh], ALU.add, reads=[t_src], writes=[t_dst])
                    P.copy(eng, dst[:, 0:sh], src[:, 0:sh], reads=[t_src], writes=[t_dst])
                    src, t_src = dst, t_dst
                    sh *= 2
                    step += 1
                p_, t_p = pl[g % 2]
                P.stt(p_[:, W:SEQ], src[:, W:SEQ], 1.0 / W, u[:, W:SEQ], ALU.mult, ALU.subtract,
                      reads=[t_src, t_u], writes=[t_p])
                tmp, t_tmp = wa[step % 2]
                P.tt("dve", tmp[:, 0:W], src[:, 0:W], rc[:, 0:W], ALU.mult, reads=[t_src, t_rc], writes=[t_tmp])
                P.tt("dve", p_[:, 0:W], tmp[:, 0:W], u[:, 0:W], ALU.subtract, reads=[t_tmp, t_u], writes=[t_p])
                for c in range(NCH):
                    pm, t_pm = pp[k % 2]
                    y, t_y = yst[k % 2]
                    k += 1
                    P.mm(pm[:], wp[:, g, :], p_[:, c * 512:(c + 1) * 512], True, True, reads=[t_wp, t_p], writes=[t_pm])
                    P.ts("dve", y[:], pm[:], psc[:, g:g + 1], None, ALU.mult, reads=[t_pm, t_psc], writes=[t_y])
                    P.dma("sp", yT_d[g * 128:(g + 1) * 128, c * 512:(c + 1) * 512], y[:], reads=[t_y])

        with (Phase(nc, "p4", G) if stop >= 4 else Skip()) as P:
            P.eps_ap = eps_t[:]
            wos, t_wosl = P.sb([128, 4, D], BF16, "wos", ntok=4)
            wop, t_wopl = P.sb([128, 4, D], BF16, "wop", ntok=4)
            gs, t_gs = P.sb([128, 4], F32, "gs")
            gp, t_gp = P.sb([128, 4], F32, "gp")
            for h in range(4):
                P.dma("pool", wos[:, h, :], w_out[h * 128:(h + 1) * 128, :], writes=[t_wosl[h]])
            for g in range(4):
                P.dma("pool", wop[:, g, :], w_out[512 + g * 128:512 + (g + 1) * 128, :], writes=[t_wopl[g]])
            P.dma("sp", gs[:], g_sb, writes=[t_gs])
            P.dma("sp", gp[:], g_pool, writes=[t_gp])
            sbc = [P.sb([128, 4, 512], F32, "sbc") for _ in range(2)]
            yc = [P.sb([128, 4, 512], F32, "yc") for _ in range(2)]
            asb = [P.sb([128, 4, 512], BF16, "asb", ntok=4) for _ in range(2)]
            apl = [P.sb([128, 4, 512], BF16, "apl", ntok=4) for _ in range(2)]
            sqs = [P.sb([128, 4, 512], BF16, "sqs") for _ in range(2)]
            sqp = [P.sb([128, 4, 512], BF16, "sqp") for _ in range(2)]
            xt = [P.sb([128, D], F32, "x") for _ in range(2)]
            ot = [P.sb([128, D], F32, "o") for _ in range(2)]
            rs = [P.sb([128, 2], F32, "rs") for _ in range(2)]
            pss = [P.ps([128, 2], F32, "pss") for _ in range(2)]
            po = [P.ps([128, D], F32, "po") for _ in range(2)]

            def prep4(c):
                (sc, t_sc), (y_, t_y), (a_s, t_as), (a_p, t_ap) = sbc[c % 2], yc[c % 2], asb[c % 2], apl[c % 2]
                (q_s, t_qs), (q_p, t_qp) = sqs[c % 2], sqp[c % 2]
                P.dma("sp", sc[:], sbT_d[:, c * 512:(c + 1) * 512].rearrange("(h d) t -> d h t", d=128), writes=[t_sc])
                P.dma("sp", y_[:], yT_d[:, c * 512:(c + 1) * 512].rearrange("(g e) t -> e g t", e=128), writes=[t_y])
                P.act(q_s[:], sc[:], AF.Square, reads=[t_sc], writes=[t_qs])
                P.act(q_p[:], y_[:], AF.Square, reads=[t_y], writes=[t_qp])
                for h in range(4):
                    if h % 2:
                        P.ts("dve", a_s[:, h, :], sc[:, h, :], gs[:, h:h + 1], None, ALU.mult, reads=[t_sc, t_gs], writes=[t_as[h]])
                    else:
                        P.act(a_s[:, h, :], sc[:, h, :], AF.Copy, reads=[t_sc, t_gs], writes=[t_as[h]], scale=gs[:, h:h + 1])
                for g in range(4):
                    if g % 2:
                        P.ts("dve", a_p[:, g, :], y_[:, g, :], gp[:, g:g + 1], None, ALU.mult, reads=[t_y, t_gp], writes=[t_ap[g]])
                    else:
                        P.act(a_p[:, g, :], y_[:, g, :], AF.Copy, reads=[t_y, t_gp], writes=[t_ap[g]], scale=gp[:, g:g + 1])

            def tile4(c, t):
                (a_s, t_as), (a_p, t_ap) = asb[c % 2], apl[c % 2]
                (q_s, t_qs), (q_p, t_qp) = sqs[c % 2], sqp[c % 2]
                ti = c * 4 + t
                tok0 = ti * 128
                tsl = slice(t * 128, (t + 1) * 128)
                xs_, t_x = xt[ti % 2]
                o_, t_o = ot[ti % 2]
                r_, t_r = rs[ti % 2]
                ps_, t_ps = pss[ti % 2]
                pz, t_pz = po[ti % 2]
                P.dma("sp", xs_[:], x[tok0:tok0 + 128, :], writes=[t_x])
                for h in range(4):
                    P.mm(ps_[:, 0:1], q_s[:, h, tsl], ones_b[:, 0:1], h == 0, h == 3, reads=[t_qs], writes=[t_ps])
                yield
                P.act(r_[:, 0:1], ps_[:, 0:1], AF.Ln, reads=[t_ps], writes=[t_r], scale=1.0 / 512, bias=eps_t[:])
                yield
                for g in range(4):
                    P.mm(ps_[:, 1:2], q_p[:, g, tsl], ones_b[:, 0:1], g == 0, g == 3, reads=[t_qp], writes=[t_ps])
                yield
                P.act(r_[:, 1:2], ps_[:, 1:2], AF.Ln, reads=[t_ps], writes=[t_r], scale=1.0 / 512, bias=eps_t[:])
                yield
                P.act(r_[:], r_[:], AF.Exp, reads=[t_r], writes=[t_r], scale=-0.5)
                for nh in range(2):
                    for h in range(4):
                        P.mm(pz[:, nh * 512:(nh + 1) * 512], a_s[:, h, tsl], wos[:, h, nh * 512:(nh + 1) * 512],
                             h == 0, h == 3, reads=[t_as[h], t_wosl[h]], writes=[t_pz])
                yield
                for nh in range(2):
                    hs = slice(nh * 512, (nh + 1) * 512)
                    P.stt(o_[:, hs], pz[:, hs], r_[:, 0:1], xs_[:, hs], ALU.mult, ALU.add, reads=[t_pz, t_r, t_x], writes=[t_o])
                yield
                for nh in range(2):
                    for g in range(4):
                        P.mm(pz[:, nh * 512:(nh + 1) * 512], a_p[:, g, tsl], wop[:, g, nh * 512:(nh + 1) * 512],
                             g == 0, g == 3, reads=[t_ap[g], t_wopl[g]], writes=[t_pz])
                yield
                for nh in range(2):
                    hs = slice(nh * 512, (nh + 1) * 512)
                    P.stt(o_[:, hs], pz[:, hs], r_[:, 1:2], o_[:, hs], ALU.mult, ALU.add, reads=[t_pz, t_r, t_o], writes=[t_o])
                yield
                P.dma("sp", x1_d[tok0:tok0 + 128, :], o_[:], reads=[t_o])
                yield

            prep4(0)
            for c in range(NCH):
                if c + 1 < NCH:
                    prep4(c + 1)
                run_interleaved((tile4(c, t) for t in range(4)), 2)

        with (Phase(nc, "p5a", G) if stop >= 5 else Skip()) as P:
            P.eps_ap = eps_t[:]
            junk, t_junk = P.sb([128, D], BF16, "junk")
            ptr = [P.ps([128, 8, 128], BF16, "ptr") for _ in range(2)]
            pmm = [P.ps([128, 512], F32, "pmm") for _ in range(3)]
            wkv, t_wkvl = P.sb([128, 8, 2 * D], BF16, "wkv", ntok=8)
            gkv, t_gkv = P.sb([128, D], F32, "gkv")
            mT, t_mT = P.sb([128, 8, NMEM], BF16, "mT")
            mt = [P.sb([128, D], F32, "m") for _ in range(2)]
            mb = [P.sb([128, D], BF16, "mb") for _ in range(2)]
            mss = [P.sb([128, 1], F32, "mss") for _ in range(2)]
            for kc in range(8):
                P.dma("pool", wkv[:, kc, :], w_mem_kv[kc * 128:(kc + 1) * 128, :], writes=[t_wkvl[kc]])
            P.dma("sp", gkv[:], g_mem_kv.partition_broadcast(128), writes=[t_gkv])
            for t in range(2):
                (m_, t_m), (b_, t_b), (s_, t_s) = mt[t], mb[t], mss[t]
                pt, t_pt = ptr[t]
                P.dma("sp", m_[:], mem[t * 128:(t + 1) * 128, :], writes=[t_m])
                P.act(junk[:], m_[:], AF.Square, reads=[t_m], writes=[t_junk, t_s], accum_out=s_[:])
                P.rstd(s_[:], s_[:], D, t_s, t_s)
                P.stt(b_[:], m_[:], s_[:, 0:1], gkv[:], ALU.mult, ALU.mult, reads=[t_m, t_s, t_gkv], writes=[t_b])
                for kc in range(8):
                    P.tr(pt[:, kc, :], b_[:, kc * 128:(kc + 1) * 128], ident_b[:], reads=[t_b], writes=[t_pt])
                P.copy("dve", mT[:, :, t * 128:(t + 1) * 128], pt[:], reads=[t_pt], writes=[t_mT])
            mi = 0
            for fc in range(8):
                pm, t_pm = pmm[mi % 3]
                mi += 1
                for kc in range(8):
                    P.mm(pm[:, 0:NMEM], wkv[:, kc, fc * 128:(fc + 1) * 128], mT[:, kc, :], kc == 0, kc == 7,
                         reads=[t_wkvl[kc], t_mT], writes=[t_pm])
                P.copy("act" if fc % 2 else "dve", KT[:, fc, :], pm[:, 0:NMEM], reads=[t_pm], writes=[])
            for t in range(2):
                for nh in range(2):
                    pm, t_pm = pmm[mi % 3]
                    mi += 1
                    for kc in range(8):
                        P.mm(pm[:], mT[:, kc, t * 128:(t + 1) * 128], wkv[:, kc, D + nh * 512:D + (nh + 1) * 512],
                             kc == 0, kc == 7, reads=[t_wkvl[kc], t_mT], writes=[t_pm])
                    P.copy("act" if nh else "dve", V[:, t, nh * 512:(nh + 1) * 512], pm[:], reads=[t_pm], writes=[])

        with (Phase(nc, "p5", G) if stop >= 5 else Skip()) as P:
            P.eps_ap = eps_t[:]
            wq, t_wql = P.sb([128, 8, D], BF16, "wq", ntok=8)
            wo, t_wol = P.sb([128, 8, D], BF16, "wo", ntok=8)
            gq, t_gq = P.sb([128, D], F32, "gq")
            for kc in range(8):
                P.dma("pool", wq[:, kc, :], w_mem_q[kc * 128:(kc + 1) * 128, :], writes=[t_wql[kc]])
            for kc in range(8):
                P.dma("pool", wo[:, kc, :], w_mem_o[kc * 128:(kc + 1) * 128, :], writes=[t_wol[kc]])
            P.dma("sp", gq[:], g_mem_q.partition_broadcast(128), writes=[t_gq])
            ptr = [P.ps([128, 8, 128], BF16, "ptr") for _ in range(2)]
            pmm = [P.ps([128, 512], F32, "pmm") for _ in range(3)]
            psc_ = [P.ps([128, NMEM], F32, "psc") for _ in range(3)]
            NU = 5
            junks = [P.sb([128, D], BF16, "junk") for _ in range(2)]
            xc = [P.sb([128, 4, D], F32, "x1c") for _ in range(3)]
            hq = [P.sb([128, D], BF16, "hq") for _ in range(8)]
            sq = [P.sb([128, 1], F32, "sq") for _ in range(4)]
            hqTb = [P.sb([128, 8, 512], BF16, "hqT") for _ in range(2)]
            qmb = [P.sb([128, 8, 512], BF16, "qm", ntok=8) for _ in range(2)]
            pf = [P.sb([128, NMEM], F32, "pf") for _ in range(NU)]
            pb = [P.sb([128, NMEM], BF16, "pb") for _ in range(NU)]
            sm = [P.sb([128, 4], F32, "sm") for _ in range(NU)]
            pT, t_pTl = P.sb([128, 2, 4, 512], BF16, "pT", ntok=4)
            oT, t_oTl = P.sb([128, 8, 512], BF16, "oT", ntok=8)
            x2t = [P.sb([128, D], F32, "x2") for _ in range(2)]
            st5 = {"mi": 0}

            def load5(c):
                xc_, t_xc = xc[c % 3]
                P.dma("sp", xc_[:], x1_d[c * 512:(c + 1) * 512, :].rearrange("(t p) d -> p t d", p=128), writes=[t_xc])

            def tileA(c, t):
                xc_, t_xc = xc[c % 3]
                ti = c * 4 + t
                (h_, t_h), (s_, t_s) = hq[ti % 8], sq[ti % 4]
                junk, t_junk = junks[ti % 2]
                P.act(junk[:], xc_[:, t, :], AF.Square, reads=[t_xc], writes=[t_junk, t_s], accum_out=s_[:])
                yield
                P.act(s_[:], s_[:], AF.Ln, reads=[t_s], writes=[t_s], scale=1.0 / D, bias=eps_t[:])
                yield
                P.act(s_[:], s_[:], AF.Exp, reads=[t_s], writes=[t_s], scale=-0.5)
                yield
                P.stt(h_[:], xc_[:, t, :], s_[:, 0:1], gq[:], ALU.mult, ALU.mult, reads=[t_xc, t_s, t_gq], writes=[t_h])
                yield

            def trA(c):
                hqT, t_hqT = hqTb[c % 2]
                for t in range(4):
                    ti = c * 4 + t
                    h_, t_h = hq[ti % 8]
                    pt, t_pt = ptr[ti % 2]
                    for kc in range(8):
                        P.tr(pt[:, kc, :], h_[:, kc * 128:(kc + 1) * 128], ident_b[:], reads=[t_h], writes=[t_pt])
                    P.copy("dve" if t % 2 else "act", hqT[:, :, t * 128:(t + 1) * 128], pt[:], reads=[t_pt], writes=[t_hqT])

            def unitC(c, t, hd):
                k = (c * 4 + t) * 4 + hd
                qm, t_qml = qmb[c % 2]
                tsl = slice(t * 128, (t + 1) * 128)
                pscr, t_pscr = psc_[k % 3]
                (p_f, t_pf), (p_b, t_pb), (m_, t_m) = pf[k % NU], pb[k % NU], sm[k % NU]
                pt, t_pt = ptr[k % 2]
                for dc in range(2):
                    P.mm(pscr[:, 0:NMEM], qm[:, hd * 2 + dc, tsl], KT[:, hd * 2 + dc, :], dc == 0, dc == 1,
                         reads=[t_qml[hd * 2 + dc]], writes=[t_pscr])
                P.S.op("dve", lambda e: e.reduce_max(out=m_[:, 0:1], in_=pscr[:, 0:NMEM], axis=mybir.AxisListType.X),
                       [t_pscr], [t_m])
                P.ts("dve", m_[:, 1:2], m_[:, 0:1], -1.0 / 16, None, ALU.mult, reads=[t_m], writes=[t_m])
                P.act(p_f[:], pscr[:, 0:NMEM], AF.Exp, reads=[t_pscr, t_m], writes=[t_pf, t_m], scale=1.0 / 16,
                      bias=m_[:, 1:2], accum_out=m_[:, 2:3])
                yield
                P.S.op("dve", lambda e: e.reciprocal(m_[:, 3:4], m_[:, 2:3]), [t_m], [t_m])
                yield
                P.ts("dve", p_b[:], p_f[:], m_[:, 3:4], None, ALU.mult, reads=[t_pf, t_m], writes=[t_pb])
                yield
                for mc in range(2):
                    P.tr(pt[:, mc, :], p_b[:, mc * 128:(mc + 1) * 128], ident_b[:], reads=[t_pb], writes=[t_pt])
                P.copy("act", pT[:, :, hd, tsl], pt[:, 0:2, :], reads=[t_pt], writes=[t_pTl[hd]])
                yield

            def projB(c, fc):
                hqT, t_hqT = hqTb[c % 2]
                qm, t_qml = qmb[c % 2]
                pm, t_pm = pmm[st5["mi"] % 3]
                st5["mi"] += 1
                for kc in range(8):
                    P.mm(pm[:], wq[:, kc, fc * 128:(fc + 1) * 128], hqT[:, kc, :], kc == 0, kc == 7,
                         reads=[t_wql[kc], t_hqT], writes=[t_pm])
                P.copy("act" if fc % 2 else "dve", qm[:, fc, :], pm[:], reads=[t_pm], writes=[t_qml[fc]])

            def unitsC(c):
                gens = [unitC(c, t, hd) for t in range(4) for hd in range(4)]
                active = []
                nxt = 0
                fc = 0
                rounds = 0
                while nxt < len(gens) or active:
                    while nxt < len(gens) and len(active) < 4:
                        active.append(gens[nxt])
                        nxt += 1
                    for g_ in list(active):
                        try:
                            next(g_)
                        except StopIteration:
                            active.remove(g_)
                    rounds += 1
                    if c + 1 < NCH and fc < 8 and rounds % 2 == 0:
                        projB(c + 1, fc)
                        fc += 1
                while c + 1 < NCH and fc < 8:
                    projB(c + 1, fc)
                    fc += 1

            load5(0)
            run_interleaved((tileA(0, t) for t in range(4)), 2)
            trA(0)
            for fc in range(8):
                projB(0, fc)
            if NCH > 1:
                load5(1)
                run_interleaved((tileA(1, t) for t in range(4)), 2)
                trA(1)
            for c in range(NCH):
                xc_, t_xc = xc[c % 3]
                unitsC(c)
                if c + 2 < NCH:
                    load5(c + 2)
                    run_interleaved((tileA(c + 2, t) for t in range(4)), 2)
                for fc in range(8):
                    hd = fc // 2
                    pm, t_pm = pmm[st5["mi"] % 3]
                    st5["mi"] += 1
                    for mc in range(2):
                        P.mm(pm[:], V[:, mc, fc * 128:(fc + 1) * 128], pT[:, mc, hd, :], mc == 0, mc == 1,
                             reads=[t_pTl[hd]], writes=[t_pm])
                    P.copy("act" if fc % 2 else "dve", oT[:, fc, :], pm[:], reads=[t_pm], writes=[t_oTl[fc]])
                for t in range(4):
                    tok0 = c * 512 + t * 128
                    tsl = slice(t * 128, (t + 1) * 128)
                    x2_, t_x2t = x2t[t % 2]
                    for nh in range(2):
                        pm, t_pm = pmm[st5["mi"] % 3]
                        st5["mi"] += 1
                        for fc in range(8):
                            P.mm(pm[:], oT[:, fc, tsl], wo[:, fc, nh * 512:(nh + 1) * 512], fc == 0, fc == 7,
                                 reads=[t_oTl[fc], t_wol[fc]], writes=[t_pm])
                        P.tt("dve", x2_[:, nh * 512:(nh + 1) * 512], pm[:], xc_[:, t, nh * 512:(nh + 1) * 512], ALU.add,
                             reads=[t_pm, t_xc], writes=[t_x2t])
                    P.dma("sp", x2_d[tok0:tok0 + 128, :], x2_[:], reads=[t_x2t])
                if c + 2 < NCH:
                    trA(c + 2)

        with (Phase(nc, "p6", G) if stop >= 6 else Skip()) as P:
            P.eps_ap = eps_t[:]
            wr, t_wr = P.sb([128, 8, NE], F32, "wr")
            br, t_br = P.sb([128, NE], F32, "br")
            gf, t_gf = P.sb([128, D], F32, "gf")
            eoff, t_eo = P.sb([128, NE], F32, "eoff")
            ltri, t_lt = P.sb([128, 128], BF16, "ltri")
            cum, t_cum = P.sb([128, NE], F32, "cum")
            cumb, t_cumb = P.sb([128, NE], BF16, "cumb")
            P.dma("sp", wr[:], w_router.rearrange("(kc p) e -> p kc e", p=128), writes=[t_wr])
            P.dma("sp", br[:], b_router.partition_broadcast(128), writes=[t_br])
            P.dma("sp", gf[:], g_ffn.partition_broadcast(128), writes=[t_gf])
            P.dma("sp", eoff[:], c_eoff, writes=[t_eo])
            P.dma("pool", ltri[:], c_ltri, writes=[t_lt])
            P.S.op("dve", lambda e: e.memset(cum[:], 0.0), [], [t_cum])
            P.S.op("dve", lambda e: e.memset(cumb[:], 0.0), [], [t_cumb])
            NB6 = 4
            junks = [P.sb([128, D], BF16, "junk") for _ in range(NB6)]
            xt = [P.sb([128, D], F32, "x") for _ in range(NB6)]
            hf = [P.sb([128, D], F32, "hf") for _ in range(NB6)]
            hbb = [P.sb([128, D], BF16, "hb") for _ in range(NB6)]
            hT3 = [P.sb([128, 8, 128], F32, "hT3") for _ in range(NB6)]
            sq = [P.sb([128, 1], F32, "sq") for _ in range(NB6)]
            lg = [P.sb([128, NE], F32, "lg") for _ in range(NB6)]
            t8 = [P.sb([128, 8], F32, "t8") for _ in range(NB6)]
            sel = [P.sb([128, NE], BF16, "sel") for _ in range(NB6)]
            self_ = [P.sb([128, NE], F32, "self") for _ in range(NB6)]
            posf = [P.sb([128, NE], F32, "posf") for _ in range(NB6)]
            sc32 = [P.sb([128, NE], F32, "sc32") for _ in range(NB6)]
            pk = [P.sb([128, 4], F32, "pk") for _ in range(NB6)]
            pki = [P.sb([128, 4], I32, "pki") for _ in range(NB6)]
            gt = [P.sb([128, 8], F32, "gt") for _ in range(NB6)]
            ptr = [P.ps([128, 8, 128], F32, "ptr") for _ in range(2)]
            plg = [P.ps([128, NE], F32, "plg") for _ in range(2)]
            prk = [P.ps([128, NE], F32, "prk") for _ in range(2)]

            def tile6(ti):
                tok0 = ti * 128
                b = ti % NB6
                (x_, t_x), (h_, t_h), (hb_, t_hb), (hT_, t_hT), (s_, t_s) = xt[b], hf[b], hbb[b], hT3[b], sq[b]
                (l_, t_l), (t8_, t_t8), (se_, t_se), (sf_, t_sf), (po_, t_po), (sc_, t_scr) = lg[b], t8[b], sel[b], self_[b], posf[b], sc32[b]
                (pk_, t_pk), (pki_, t_pki), (g_, t_g) = pk[b], pki[b], gt[b]
                (pt, t_pt), (pl_, t_pl), (pr_, t_pr) = ptr[ti % 2], plg[ti % 2], prk[ti % 2]
                junk, t_junk = junks[b]
                P.dma("act", x_[:], x2_d[tok0:tok0 + 128, :], writes=[t_x])
                yield
                P.act(junk[:], x_[:], AF.Square, reads=[t_x], writes=[t_junk, t_s], accum_out=s_[:])
                yield
                P.act(s_[:], s_[:], AF.Ln, reads=[t_s], writes=[t_s], scale=1.0 / D, bias=eps_t[:])
                yield
                P.act(s_[:], s_[:], AF.Exp, reads=[t_s], writes=[t_s], scale=-0.5)
                yield
                P.stt(h_[:], x_[:], s_[:, 0:1], gf[:], ALU.mult, ALU.mult, reads=[t_x, t_s, t_gf], writes=[t_h])
                yield
                P.copy("act", hb_[:], h_[:], reads=[t_h], writes=[t_hb])
                for kc in range(8):
                    P.tr(pt[:, kc, :], h_[:, kc * 128:(kc + 1) * 128], ident_f[:], reads=[t_h], writes=[t_pt])
                P.copy("dve", hT_[:, 0:4, :], pt[:, 0:4, :], reads=[t_pt], writes=[t_hT])
                P.copy("act", hT_[:, 4:8, :], pt[:, 4:8, :], reads=[t_pt], writes=[t_hT])
                yield
                for kc in range(8):
                    P.mm(pl_[:, 0:NE], hT_[:, kc, :], wr[:, kc, :], kc == 0, kc == 7, reads=[t_hT, t_wr], writes=[t_pl])
                P.tt("dve", l_[:], pl_[:, 0:NE], br[:], ALU.add, reads=[t_pl, t_br], writes=[t_l])
                yield
                P.S.op("dve", lambda e, t8_=t8_, l_=l_: e.max(t8_[:], l_[:]), [t_l], [t_t8])
                yield
                P.ts("dve", sf_[:], l_[:], t8_[:, 3:4], None, ALU.is_ge, reads=[t_l, t_t8], writes=[t_sf])
                P.ts("pool", g_[:, 4:5], t8_[:, 0:1], -1.0, None, ALU.mult, reads=[t_t8], writes=[t_g])
                yield
                P.copy("dve", se_[:], sf_[:], reads=[t_sf], writes=[t_se])
                P.act(g_[:, 0:4], t8_[:, 0:4], AF.Exp, reads=[t_t8, t_g], writes=[t_g], bias=g_[:, 4:5], accum_out=g_[:, 5:6])
                yield
                P.mm(pr_[:, 0:NE], ltri[:], se_[:], True, False, reads=[t_lt, t_se], writes=[t_pr])
                P.mm(pr_[:, 0:NE], ones_b[:], cumb[:], False, True, reads=[t_cumb], writes=[t_pr])
                P.tt("dve", cum[:], cum[:], sf_[:], ALU.add, reads=[t_cum, t_sf], writes=[t_cum])
                P.copy("dve", cumb[:], cum[:], reads=[t_cum], writes=[t_cumb])
                P.ts("dve", sc_[:], pr_[:, 0:NE], float(CAP), 1.0e6, ALU.is_ge, ALU.mult, reads=[t_pr], writes=[t_scr])
                P.tt("dve", po_[:], pr_[:, 0:NE], eoff[:], ALU.add, reads=[t_pr, t_eo], writes=[t_po])
                yield
                P.S.op("dve", lambda e, g_=g_: e.reciprocal(g_[:, 6:7], g_[:, 5:6]), [t_g], [t_g])
                yield
                P.ts("dve", g_[:, 0:4], g_[:, 0:4], g_[:, 6:7], None, ALU.mult, reads=[t_g], writes=[t_g])
                yield
                P.dma("sp", gate_d[tok0:tok0 + 128, :], g_[:, 0:4], reads=[t_g])
                P.tt("dve", po_[:], po_[:], sc_[:], ALU.add, reads=[t_po, t_scr], writes=[t_po])
                yield
                P.ts("dve", po_[:], po_[:], float(TRASH), None, ALU.min, reads=[t_po], writes=[t_po])
                yield
                for k in range(4):
                    P.stt(sc_[:], l_[:], t8_[:, k:k + 1], po_[:], ALU.is_equal, ALU.mult, reads=[t_l, t_t8, t_po],
                          writes=[t_scr, t_pk], accum_out=pk_[:, k:k + 1])
                    yield
                P.copy("dve", pki_[:], pk_[:], reads=[t_pk], writes=[t_pki])
                yield
                P.dma("sp", pos_d[tok0:tok0 + 128, :], pki_[:], reads=[t_pki])
                for k in range(4):
                    P.S.dma("pool", lambda e, pki_=pki_, hb_=hb_, k=k: e.indirect_dma_start(
                        out=xs_d, out_offset=bass.IndirectOffsetOnAxis(ap=pki_[:, k:k + 1], axis=0),
                        in_=hb_[:], in_offset=None), [t_pki, t_hb], [])
                yield

            run_interleaved((tile6(ti) for ti in range(NT)), 3)

        with (Phase(nc, "p7", G) if stop >= 7 else Skip()) as P:
            w1b = [P.sb([128, 8, 2 * D], BF16, "w1b", ntok=8) for _ in range(2)]
            w2b = [P.sb([128, 8, D], BF16, "w2b", ntok=8) for _ in range(2)]
            b1t = [P.sb([128, 16], F32, "b1t") for _ in range(2)]
            b2t = [P.sb([128, D], F32, "b2t") for _ in range(2)]
            xr = [P.sb([128, CT, D], BF16, "xr") for _ in range(2)]
            xTb = [P.sb([128, 8, CAP], BF16, "xT") for _ in range(2)]
            NBUF = 3
            gtb = [P.sb([128, NH], F32, "gtb") for _ in range(NBUF)]
            sgb = [P.sb([128, NH], F32, "sgb") for _ in range(NBUF)]
            ubb = [P.sb([128, NH], F32, "ubb") for _ in range(NBUF)]
            aT = [P.sb([128, 8, NH], BF16, "aT") for _ in range(2)]
            yo = [P.sb([128, D], BF16, "yo") for _ in range(3)]
            ptr = [P.ps([128, 8, 128], BF16, "ptr") for _ in range(2)]
            pg = [P.ps([128, NH], F32, "pg") for _ in range(2)]
            pu = [P.ps([128, NH], F32, "pu") for _ in range(2)]
            py = [P.ps([128, 512], F32, "py") for _ in range(2)]
            st7 = {"gi": 0, "yi": 0, "pyi": 0}

            def load(ex):
                (w1_, t_w1), (w2_, t_w2), (b1_, t_b1), (b2_, t_b2), (xr_, t_xr) = \
                    w1b[ex % 2], w2b[ex % 2], b1t[ex % 2], b2t[ex % 2], xr[ex % 2]
                P.dma("sp", xr_[:], xs_d[ex * CAP:(ex + 1) * CAP, :].rearrange("(t p) d -> p t d", p=128), writes=[t_xr])
                for kc in range(8):
                    P.dma("pool", w1_[:, kc, :], w1[ex, kc * 128:(kc + 1) * 128, :], writes=[t_w1[kc]])
                for kc in range(8):
                    P.dma("pool", w2_[:, kc, :], w2[ex, kc * 128:(kc + 1) * 128, :], writes=[t_w2[kc]])
                P.dma("sp", b1_[:], b1[ex], writes=[t_b1])
                P.dma("sp", b2_[:], b2[ex].partition_broadcast(128), writes=[t_b2])

            def transposes(ex):
                xr_, t_xr = xr[ex % 2]
                xT, t_xT = xTb[ex % 2]
                for t in range(CT):
                    pt, t_pt = ptr[t % 2]
                    for kc in range(8):
                        P.tr(pt[:, kc, :], xr_[:, t, kc * 128:(kc + 1) * 128], ident_b[:], reads=[t_xr], writes=[t_pt])
                    P.copy("act" if t % 2 else "dve", xT[:, :, t * 128:(t + 1) * 128], pt[:], reads=[t_pt], writes=[t_xT])

            def pair_front(ex, sh, j):
                (w1_, t_w1), (b1_, t_b1) = w1b[ex % 2], b1t[ex % 2]
                xT, t_xT = xTb[ex % 2]
                a_, t_a = aT[(ex * NHALF + sh) % 2]
                ssl = slice(sh * NH, (sh + 1) * NH)
                gi = st7["gi"]
                st7["gi"] += 1
                (pg_, t_pg), (pu_, t_pu) = pg[gi % 2], pu[gi % 2]
                (g_, t_g), (s_, t_s), (u_, t_u) = gtb[gi % NBUF], sgb[gi % NBUF], ubb[gi % NBUF]
                for kc in range(8):
                    P.mm(pg_[:, 0:NH], w1_[:, kc, j * 128:(j + 1) * 128], xT[:, kc, ssl], kc == 0, kc == 7,
                         reads=[t_w1[kc], t_xT], writes=[t_pg])
                for kc in range(8):
                    P.mm(pu_[:, 0:NH], w1_[:, kc, D + j * 128:D + (j + 1) * 128], xT[:, kc, ssl], kc == 0, kc == 7,
                         reads=[t_w1[kc], t_xT], writes=[t_pu])
                P.ts("dve", g_[:], pg_[:, 0:NH], b1_[:, j:j + 1], 7.0, ALU.add, ALU.min, reads=[t_pg, t_b1], writes=[t_g])
                P.act(u_[:], pu_[:, 0:NH], AF.Identity, reads=[t_pu, t_b1], writes=[t_u], bias=b1_[:, 8 + j:9 + j])
                P.act(s_[:], g_[:], AF.Sigmoid, reads=[t_g], writes=[t_s], scale=1.702)
                P.ts("pool", u_[:], u_[:], 7.0, -7.0, ALU.min, ALU.max, reads=[t_u], writes=[t_u])
                P.tt("pool", s_[:], g_[:], s_[:], ALU.mult, reads=[t_g, t_s], writes=[t_s])

                def back():
                    P.stt(a_[:, j, :], u_[:], 1.0, s_[:], ALU.add, ALU.mult, reads=[t_u, t_s], writes=[t_a])
                return back

            def ymm(ex, sh):
                (w2_, t_w2), (b2_, t_b2) = w2b[ex % 2], b2t[ex % 2]
                a_, t_a = aT[(ex * NHALF + sh) % 2]
                for t in range(NH // 128):
                    row0 = ex * CAP + sh * NH + t * 128
                    y_, t_y = yo[st7["yi"] % 3]
                    st7["yi"] += 1
                    for nh in range(2):
                        py_, t_py = py[st7["pyi"] % 2]
                        st7["pyi"] += 1
                        for j in range(8):
                            P.mm(py_[:], a_[:, j, t * 128:(t + 1) * 128], w2_[:, j, nh * 512:(nh + 1) * 512], j == 0, j == 7,
                                 reads=[t_a, t_w2[j]], writes=[t_py])
                        P.tt("dve", y_[:, nh * 512:(nh + 1) * 512], py_[:], b2_[:, nh * 512:(nh + 1) * 512], ALU.add,
                             reads=[t_py, t_b2], writes=[t_y])
                    P.dma("sp", ys_d[row0:row0 + 128, :], y_[:], reads=[t_y])

            load(0)
            transposes(0)
            prev_unit = None
            for ex in range(NE):
                for sh in range(NHALF):
                    pend = None
                    for j in range(8):
                        back = pair_front(ex, sh, j)
                        if pend is not None:
                            pend()
                        pend = back
                        if j == 1 and prev_unit is not None:
                            ymm(*prev_unit)
                            prev_unit = None
                        if j == 1 and sh == 0 and ex + 1 < NE:
                            load(ex + 1)
                        if j == 4 and sh == NHALF - 1 and ex + 1 < NE:
                            transposes(ex + 1)
                    pend()
                    if prev_unit is not None:
                        ymm(*prev_unit)
                    prev_unit = (ex, sh)
            ymm(*prev_unit)

        with (Phase(nc, "p8", G) if stop >= 8 else Skip()) as P:
            P.eps_ap = eps_t[:]
            gfin, t_gfin = P.sb([128, D], F32, "gfin")
            P.dma("sp", gfin[:], g_final.partition_broadcast(128), writes=[t_gfin])
            NB8 = 7
            junks = [P.sb([128, D], BF16, "junk") for _ in range(NB8)]
            xt = [P.sb([128, D], F32, "x") for _ in range(NB8)]
            yk = [[P.sb([128, D], BF16, "yk") for _ in range(4)] for _ in range(NB8)]
            pki = [P.sb([128, 4], I32, "pki") for _ in range(NB8)]
            gt = [P.sb([128, 4], F32, "gt") for _ in range(NB8)]
            sq = [P.sb([128, 1], F32, "sq") for _ in range(NB8)]
            ot = [P.sb([128, D], F32, "o") for _ in range(NB8)]

            def tile8(ti):
                tok0 = ti * 128
                b = ti % NB8
                (x_, t_x), (pki_, t_pki), (g_, t_g), (s_, t_s), (o_, t_o) = xt[b], pki[b], gt[b], sq[b], ot[b]
                junk, t_junk = junks[b]
                P.dma("act", pki_[:], pos_d[tok0:tok0 + 128, :], writes=[t_pki])
                P.dma("act", x_[:], x2_d[tok0:tok0 + 128, :], writes=[t_x])
                P.dma("act", g_[:], gate_d[tok0:tok0 + 128, :], writes=[t_g])
                yield
                for k in range(4):
                    y_, t_y = yk[b][k]
                    P.S.dma("pool", lambda e, pki_=pki_, y_=y_, k=k: e.indirect_dma_start(
                        out=y_[:], out_offset=None, in_=ys_d,
                        in_offset=bass.IndirectOffsetOnAxis(ap=pki_[:, k:k + 1], axis=0)), [t_pki], [t_y])
                yield
                for k in range(4):
                    y_, t_y = yk[b][k]
                    P.stt(x_[:], y_[:], g_[:, k:k + 1], x_[:], ALU.mult, ALU.add, reads=[t_y, t_g, t_x], writes=[t_x])
                    yield
                P.act(junk[:], x_[:], AF.Square, reads=[t_x], writes=[t_junk, t_s], accum_out=s_[:])
                yield
                P.act(s_[:], s_[:], AF.Ln, reads=[t_s], writes=[t_s], scale=1.0 / D, bias=eps_t[:])
                yield
                P.act(s_[:], s_[:], AF.Exp, reads=[t_s], writes=[t_s], scale=-0.5)
                yield
                P.stt(o_[:], x_[:], s_[:, 0:1], gfin[:], ALU.mult, ALU.mult, reads=[t_x, t_s, t_gfin], writes=[t_o])
                yield
                P.dma("sp", out[tok0:tok0 + 128, :], o_[:], reads=[t_o])
                yield

            run_interleaved((tile8(ti) for ti in range(NT)), 6)
    return nc


def make_consts(CAP):
    j = np.arange(128)[:, None]
    s = np.arange(128)[None, :]
    c = {}
    c["c_ident"] = np.eye(128, dtype=np.float32)
    c["c_tri1"] = np.where(j >= s, -1.0, 0.0).astype(np.float32)
    c["c_tri2"] = np.where(j < s, -1.0, 0.0).astype(np.float32)
    c["c_ltri"] = np.where(j < s, 1.0, 0.0).astype(np.float32)
    c["c_ones"] = np.ones((128, 128), np.float32)
    m = np.zeros((4, 128, 512), np.float32)
    for jj in range(4):
        for cb in range(4):
            if cb > jj:
                m[jj, :, cb * 128:(cb + 1) * 128] = 1.0
            elif cb == jj:
                m[jj, :, cb * 128:(cb + 1) * 128] = (j < s).astype(np.float32)
    c["c_mask"] = m
    c["c_eoff"] = np.broadcast_to((np.arange(NE) * CAP).astype(np.float32)[None, :], (128, NE)).copy()
    c["c_rcnt"] = np.broadcast_to((1.0 / (np.arange(16) + 1)).astype(np.float32)[None, :], (128, 16)).copy()
    return c


def make_in_maps(inputs, n_cores, SEQ, CAP):
    f = lambda a: np.ascontiguousarray(np.asarray(a, dtype=np.float32))
    I = {k: np.asarray(v) for k, v in inputs.items()}
    shared = dict(make_consts(CAP))
    shared["g_mix"] = f(I["g_mix"][0][None, :])
    shared["w_in"] = f(I["w_in"][0])
    shared["g_sb"] = f(I["g_sb_out"][0].reshape(4, 128).T)
    shared["g_pool"] = f(I["g_pool_out"][0].reshape(4, 128).T)
    shared["w_pool"] = f(I["w_pool"][0])
    shared["pool_scale"] = f(I["pool_scale"][0].reshape(4, 128).T)
    shared["w_out"] = f(I["w_out"][0])
    shared["g_mem_q"] = f(I["g_mem_q"][0][None, :])
    shared["g_mem_kv"] = f(I["g_mem_kv"][0][None, :])
    shared["w_mem_q"] = f(I["w_mem_q"][0])
    shared["w_mem_kv"] = f(I["w_mem_kv"][0])
    shared["w_mem_o"] = f(I["w_mem_o"][0])
    shared["g_ffn"] = f(I["g_ffn"][0][None, :])
    shared["w_router"] = f(I["w_router"][0])
    shared["b_router"] = f(I["b_router"][0][None, :])
    shared["w1"] = f(I["w_expert_in"][0])
    shared["b1"] = f(I["b_expert_in"][0].reshape(NE, 16, 128).transpose(0, 2, 1))
    shared["w2"] = f(I["w_expert_out"][0])
    shared["b2"] = f(I["b_expert_out"][0][:, None, :])
    shared["g_final"] = f(I["g_final"][None, :])
    maps = []
    for c in range(n_cores):
        m = dict(shared)
        m["x"] = f(I["x"][c, :SEQ])
        m["mem"] = f(I["mem"][c])
        maps.append(m)
    return maps


def kernel(**inputs):
    SEQ, CAP = 4096, 768
    n = 8
    nc = build(SEQ, CAP)
    maps = make_in_maps(inputs, n, SEQ, CAP)
    res = run_bass_kernel_spmd(nc, maps, core_ids=list(range(n)))
    return np.stack([np.asarray(r["out"], dtype=np.float32) for r in res.results], axis=0)
```

```python
import contextlib
import numpy as np
import concourse.bass as bass
import concourse.mybir as mybir
from concourse.bass_utils import run_bass_kernel_spmd

F32 = mybir.dt.float32
BF16 = mybir.dt.bfloat16
I32 = mybir.dt.int32
AF = mybir.ActivationFunctionType
ALU = mybir.AluOpType

N_DMA_SEMS = 32
D = 1024
NE = 32
NMEM = 256
EPS = 1e-5


class Tok:
    __slots__ = ("last_w", "readers")

    def __init__(self):
        self.last_w = None
        self.readers = []


class Op:
    __slots__ = ("eng", "fn", "deps", "signaled", "sem", "val", "is_dma")

    def __init__(self, eng, fn, is_dma):
        self.eng = eng
        self.fn = fn
        self.deps = []
        self.signaled = False
        self.sem = None
        self.val = 0
        self.is_dma = is_dma


class Sched:
    ENGS = ("pe", "act", "dve", "pool", "sp")

    def __init__(self, G):
        self.G = G
        self.ops = {e: [] for e in self.ENGS}
        self.dma_rr = {"sp": 0, "pool": 0, "act": 0}
        self.dma_last = [None] * N_DMA_SEMS
        self.dma_cnt = G["dma_cnt"]

    def _add(self, eng, fn, reads, writes, is_dma):
        op = Op(eng, fn, is_dma)
        deps = []
        for t in reads:
            if t.last_w is not None:
                deps.append(t.last_w)
        for t in writes:
            if t.last_w is not None:
                deps.append(t.last_w)
            deps.extend(t.readers)
        if is_dma:
            half = N_DMA_SEMS // 2
            j = self.dma_rr[eng] + (half if eng == "pool" else 0)
            self.dma_rr[eng] = (self.dma_rr[eng] + 1) % half
            if self.dma_last[j] is not None:
                deps.append(self.dma_last[j])
            self.dma_last[j] = op
            self.dma_cnt[j] += 16
            op.sem = ("dma", j)
            op.val = self.dma_cnt[j]
            op.signaled = True
        seen = set()
        for d in deps:
            if id(d) in seen:
                continue
            seen.add(id(d))
            if (not d.is_dma) and (not is_dma) and d.eng == eng and eng == "pe":
                continue
            op.deps.append(d)
            if not d.is_dma:
                d.signaled = True
        for t in reads:
            t.readers.append(op)
        for t in writes:
            t.last_w = op
            t.readers = []
        self.ops[eng].append(op)
        return op

    def op(self, eng, fn, reads=(), writes=()):
        return self._add(eng, fn, reads, writes, False)

    def dma(self, eng, fn, reads=(), writes=()):
        return self._add(eng, fn, reads, writes, True)

    def emit(self, nc, tag):
        for e in self.ENGS:
            for op in reversed(self.ops[e]):
                if not op.is_dma:
                    op.signaled = True
                    break
        for e in self.ENGS:
            c = self.G["eng_cnt"][e]
            for op in self.ops[e]:
                if op.is_dma:
                    continue
                if op.signaled:
                    c += 1
                    op.sem = ("eng", e)
                    op.val = c
            self.G["eng_cnt"][e] = c
        with contextlib.ExitStack() as st:
            sems = self.G["sems"]
            block = st.enter_context(nc.Block())
            lasts = []
            for j in range(N_DMA_SEMS):
                if self.dma_last[j] is not None:
                    lasts.append(self.dma_last[j])
            for e in self.ENGS:
                for op in reversed(self.ops[e]):
                    if not op.is_dma:
                        lasts.append(op)
                        break
            for e in self.ENGS:
                fin = Op(e, None, False)
                fin.deps = list(lasts)
                self.ops[e].append(fin)

            def run(engname, eng):
                waited = {}
                for op in self.ops[engname]:
                    need = {}
                    for d in op.deps:
                        if need.get(d.sem, 0) < d.val:
                            need[d.sem] = d.val
                    for s, v in need.items():
                        if waited.get(s, 0) < v:
                            eng.wait_ge(sems[s], v)
                            waited[s] = v
                    if op.fn is None:
                        continue
                    ins = op.fn(eng)
                    if op.is_dma:
                        ins.then_inc(sems[op.sem], 16)
                    elif op.signaled:
                        ins.then_inc(sems[op.sem], 1)

            block.tensor(lambda eng: run("pe", eng))
            block.scalar(lambda eng: run("act", eng))
            block.vector(lambda eng: run("dve", eng))
            block.gpsimd(lambda eng: run("pool", eng))
            block.sync(lambda eng: run("sp", eng))


class _Any:
    def __getattr__(self, k):
        return self

    def __call__(self, *a, **k):
        return self

    def __getitem__(self, k):
        return self

    def __iter__(self):
        return iter((self, self))


class Skip:
    def __enter__(self):
        return _Any()

    def __exit__(self, *a):
        return False


def run_interleaved(gens, width):
    active = []
    it = iter(gens)
    done = False
    while True:
        while not done and len(active) < width:
            g = next(it, None)
            if g is None:
                done = True
                break
            active.append(g)
        if not active:
            break
        for g in list(active):
            try:
                next(g)
            except StopIteration:
                active.remove(g)


class Phase:
    def __init__(self, nc, tag, G):
        self.nc = nc
        self.tag = tag
        self.S = Sched(G)
        self.st = contextlib.ExitStack()
        self.n = 0

    def __enter__(self):
        self.st.__enter__()
        return self

    def __exit__(self, *a):
        if a[0] is None:
            self.S.emit(self.nc, self.tag)
        return self.st.__exit__(*a)

    def sb(self, shape, dt, name=None, ntok=None):
        self.n += 1
        t = self.st.enter_context(self.nc.sbuf_tensor("%s_%s%d" % (self.tag, name or "t", self.n), list(shape), dt))
        if ntok is not None:
            return t, [Tok() for _ in range(ntok)]
        return t, Tok()

    def ps(self, shape, dt, name=None):
        self.n += 1
        esz = 2 if dt == BF16 else 4
        free = 1
        for d_ in shape[1:]:
            free *= d_
        nbytes = free * esz
        assert nbytes % 2048 == 0 or len(shape) == 2, shape
        shape = list(shape)
        if nbytes % 2048 != 0:
            shape[1] = ((nbytes + 2047) // 2048) * 2048 // esz
        shape[0] = 128
        t = self.st.enter_context(self.nc.psum_tensor("%s_%s%d" % (self.tag, name or "p", self.n), shape, dt))
        return t, Tok()

    def dma(self, q, out, in_, reads=(), writes=()):
        return self.S.dma(q, lambda e: e.dma_start(out=out, in_=in_), reads, writes)

    def mm(self, out, lhsT, rhs, start, stop, reads=(), writes=()):
        return self.S.op("pe", lambda e: e.matmul(out, lhsT, rhs, start=start, stop=stop), reads, writes)

    def tr(self, out, in_, ident, reads=(), writes=()):
        return self.S.op("pe", lambda e: e.transpose(out, in_, ident), reads, writes)

    def act(self, out, in_, func, reads=(), writes=(), **kw):
        return self.S.op("act", lambda e: e.activation(out, in_, func, **kw), reads, writes)

    def copy(self, eng, out, in_, reads=(), writes=()):
        if eng == "act":
            return self.S.op("act", lambda e: e.copy(out, in_), reads, writes)
        return self.S.op(eng, lambda e: e.tensor_copy(out, in_), reads, writes)

    def tt(self, eng, out, a, b, op, reads=(), writes=()):
        return self.S.op(eng, lambda e: e.tensor_tensor(out=out, in0=a, in1=b, op=op), reads, writes)

    def ts(self, eng, out, a, s1, s2, op0, op1=None, reads=(), writes=(), accum_out=None):
        if op1 is None:
            return self.S.op(eng, lambda e: e.tensor_scalar(out=out, in0=a, scalar1=s1, scalar2=None, op0=op0), reads, writes)
        if accum_out is not None:
            return self.S.op(eng, lambda e: e.tensor_scalar(out=out, in0=a, scalar1=s1, scalar2=s2, op0=op0, op1=op1,
                                                            accum_out=accum_out), reads, writes)
        return self.S.op(eng, lambda e: e.tensor_scalar(out=out, in0=a, scalar1=s1, scalar2=s2, op0=op0, op1=op1), reads, writes)

    def stt(self, out, in0, scalar, in1, op0, op1, reads=(), writes=(), accum_out=None):
        if accum_out is not None:
            return self.S.op("dve", lambda e: e.scalar_tensor_tensor(out=out, in0=in0, scalar=scalar, in1=in1, op0=op0,
                                                                     op1=op1, accum_out=accum_out), reads, writes)
        return self.S.op("dve", lambda e: e.scalar_tensor_tensor(out=out, in0=in0, scalar=scalar, in1=in1, op0=op0, op1=op1),
                         reads, writes)

    def rstd(self, rs, ss, n, t_ss, t_rs):
        self.act(rs, ss, AF.Ln, reads=[t_ss], writes=[t_rs], scale=1.0 / n, bias=self.eps_ap)
        self.act(rs, rs, AF.Exp, reads=[t_rs], writes=[t_rs], scale=-0.5)


def build(SEQ, CAP, dbg=False, stop=99):
    NT = SEQ // 128
    NCH = SEQ // 512
    NSLOT = NE * CAP
    TRASH = NSLOT
    CT = CAP // 128
    NH = CAP // 2
    assert NH <= 512 and CAP % 256 == 0 or CT == 1
    if CT == 1:
        NH = CAP
    NHALF = CAP // NH

    nc = bass.Bass("TRN2", target_bir_lowering=False)

    def din(name, shape, dt=F32):
        return nc.dram_tensor(name, list(shape), dt, kind="ExternalInput").ap()

    def dscr(name, shape, dt=F32):
        return nc.dram_tensor(name, list(shape), dt, kind="ExternalOutput" if dbg else "Internal").ap()

    x = din("x", [SEQ, D])
    mem = din("mem", [NMEM, D])
    g_mix = din("g_mix", [1, D])
    w_in = din("w_in", [D, 2048])
    g_sb = din("g_sb", [128, 4])
    g_pool = din("g_pool", [128, 4])
    w_pool = din("w_pool", [4, 128, 128])
    pool_scale = din("pool_scale", [128, 4])
    w_out = din("w_out", [D, D])
    g_mem_q = din("g_mem_q", [1, D])
    g_mem_kv = din("g_mem_kv", [1, D])
    w_mem_q = din("w_mem_q", [D, D])
    w_mem_kv = din("w_mem_kv", [D, 2 * D])
    w_mem_o = din("w_mem_o", [D, D])
    g_ffn = din("g_ffn", [1, D])
    w_router = din("w_router", [D, NE])
    b_router = din("b_router", [1, NE])
    w1 = din("w1", [NE, D, 2 * D])
    b1 = din("b1", [NE, 128, 16])
    w2 = din("w2", [NE, D, D])
    b2 = din("b2", [NE, 1, D])
    g_final = din("g_final", [1, D])
    c_ident = din("c_ident", [128, 128])
    c_tri1 = din("c_tri1", [128, 128])
    c_tri2 = din("c_tri2", [128, 128])
    c_ltri = din("c_ltri", [128, 128])
    c_ones = din("c_ones", [128, 128])
    c_mask = din("c_mask", [4, 128, 512])
    c_eoff = din("c_eoff", [128, NE])
    c_rcnt = din("c_rcnt", [128, 16])
    out = nc.dram_tensor("out", [SEQ, D], F32, kind="ExternalOutput").ap()

    uT_d = dscr("uT_d", [512, SEQ])
    sbT_d = dscr("sbT_d", [512, SEQ])
    yT_d = dscr("yT_d", [512, SEQ])
    x1_d = dscr("x1_d", [SEQ, D])
    x2_d = dscr("x2_d", [SEQ, D])
    xs_d = dscr("xs_d", [NSLOT + 128, D], BF16)
    ys_d = dscr("ys_d", [NSLOT + 128, D], BF16)
    pos_d = dscr("pos_d", [SEQ, 4], I32)
    gate_d = dscr("gate_d", [SEQ, 4])
    t_uT, t_sbT, t_yT, t_x1, t_x2, t_xs, t_ys, t_pos, t_gate = [Tok() for _ in range(9)]

    with contextlib.ExitStack() as top:
        def gsb(name, shape, dt):
            return top.enter_context(nc.sbuf_tensor(name, list(shape), dt))
        G = {"sems": {}, "eng_cnt": {e: 0 for e in Sched.ENGS}, "dma_cnt": [0] * N_DMA_SEMS}
        for e in Sched.ENGS:
            G["sems"][("eng", e)] = top.enter_context(nc.semaphore("s_" + e))
        for j in range(N_DMA_SEMS):
            G["sems"][("dma", j)] = top.enter_context(nc.semaphore("s_d%d" % j))
        eps_t = gsb("eps_t", [128, 1], F32)
        ident_f = gsb("ident_f", [128, 128], F32)
        ident_b = gsb("ident_b", [128, 128], BF16)
        ones_b = gsb("ones_b", [128, 128], BF16)
        KT = gsb("KT", [128, 8, NMEM], BF16)
        V = gsb("V", [128, 2, D], BF16)
        qkv = contextlib.ExitStack()
        qT = qkv.enter_context(nc.sbuf_tensor("qT", [128, 4, SEQ], BF16))
        kT = qkv.enter_context(nc.sbuf_tensor("kT", [128, 4, SEQ], BF16))
        vv = qkv.enter_context(nc.sbuf_tensor("vv", [128, NT, 512], BF16))

        with Phase(nc, "p0", G) as P:
            P.S.op("dve", lambda e: e.memset(eps_t[:], EPS))
            P.dma("sp", ident_f[:], c_ident)
            P.dma("pool", ident_b[:], c_ident)
            P.dma("pool", ones_b[:], c_ones)

        with (Phase(nc, "p1", G) if stop >= 1 else Skip()) as P:
            P.eps_ap = eps_t[:]
            w_bf, t_wl = P.sb([128, 8, 2048], BF16, "win", ntok=8)
            gbc, t_gbc = P.sb([128, D], F32, "gbc")
            for kc in range(8):
                P.dma("pool", w_bf[:, kc, :], w_in[kc * 128:(kc + 1) * 128, :], writes=[t_wl[kc]])
            P.dma("sp", gbc[:], g_mix.partition_broadcast(128), writes=[t_gbc])
            xt = [P.sb([128, D], F32, "x") for _ in range(3)]
            junks = [P.sb([128, D], BF16, "junk") for _ in range(2)]
            hb = [P.sb([128, D], BF16, "h") for _ in range(8)]
            ss = [P.sb([128, 1], F32, "ss") for _ in range(4)]
            hT = [P.sb([128, 8, 512], BF16, "hT") for _ in range(2)]
            ust = [P.sb([128, 512], F32, "ust") for _ in range(2)]
            ptr = [P.ps([128, 8, 128], BF16, "ptr") for _ in range(2)]
            pmm = [P.ps([128, 512], F32, "pmm") for _ in range(4)]
            st1 = {"mi": 0}

            def tileA1(c, t):
                ti = c * 4 + t
                tok0 = ti * 128
                xs_, t_x = xt[ti % 3]
                h_, t_h = hb[ti % 8]
                s_, t_s = ss[ti % 4]
                junk, t_junk = junks[ti % 2]
                P.dma("sp", xs_[:], x[tok0:tok0 + 128, :], writes=[t_x])
                yield
                P.act(junk[:], xs_[:], AF.Square, reads=[t_x], writes=[t_junk, t_s], accum_out=s_[:])
                yield
                P.act(s_[:], s_[:], AF.Ln, reads=[t_s], writes=[t_s], scale=1.0 / D, bias=eps_t[:])
                yield
                P.act(s_[:], s_[:], AF.Exp, reads=[t_s], writes=[t_s], scale=-0.5)
                yield
                P.stt(h_[:], xs_[:], s_[:, 0:1], gbc[:], ALU.mult, ALU.mult, reads=[t_x, t_s, t_gbc], writes=[t_h])
                yield

            def prepB1(c):
                hTc, t_hT = hT[c % 2]
                for t in range(4):
                    ti = c * 4 + t
                    h_, t_h = hb[ti % 8]
                    pt, t_pt = ptr[ti % 2]
                    for kc in range(8):
                        P.tr(pt[:, kc, :], h_[:, kc * 128:(kc + 1) * 128], ident_b[:], reads=[t_h], writes=[t_pt])
                    P.copy("dve" if t % 2 else "act", hTc[:, :, t * 128:(t + 1) * 128], pt[:], reads=[t_pt], writes=[t_hT])

            def mm1(c):
                hTc, t_hT = hT[c % 2]
                for j in range(12):
                    col = j * 128 if j < 8 else 1536 + (j - 8) * 128
                    pm, t_pm = pmm[st1["mi"] % 4]
                    st1["mi"] += 1
                    for kc in range(8):
                        P.mm(pm[:], w_bf[:, kc, col:col + 128], hTc[:, kc, :], kc == 0, kc == 7,
                             reads=[t_wl[kc], t_hT], writes=[t_pm])
                    if j < 4:
                        P.copy("act", qT[:, j, c * 512:(c + 1) * 512], pm[:], reads=[t_pm], writes=[])
                    elif j < 8:
                        P.copy("dve", kT[:, j - 4, c * 512:(c + 1) * 512], pm[:], reads=[t_pm], writes=[])
                    else:
                        us, t_us = ust[j % 2]
                        P.copy("act" if j % 2 else "dve", us[:], pm[:], reads=[t_pm], writes=[t_us])
                        P.dma("sp", uT_d[(j - 8) * 128:(j - 7) * 128, c * 512:(c + 1) * 512], us[:], reads=[t_us])
                for t in range(4):
                    pm, t_pm = pmm[st1["mi"] % 4]
                    st1["mi"] += 1
                    for kc in range(8):
                        P.mm(pm[:], hTc[:, kc, t * 128:(t + 1) * 128], w_bf[:, kc, 1024:1536], kc == 0, kc == 7,
                             reads=[t_wl[kc], t_hT], writes=[t_pm])
                    P.copy("act" if t % 2 else "dve", vv[:, c * 4 + t, :], pm[:], reads=[t_pm], writes=[])

            run_interleaved((tileA1(0, t) for t in range(4)), 2)
            prepB1(0)
            for c in range(NCH):
                if c + 1 < NCH:
                    run_interleaved((tileA1(c + 1, t) for t in range(4)), 2)
                mm1(c)
                if c + 1 < NCH:
                    prepB1(c + 1)

        with (Phase(nc, "p2", G) if stop >= 2 else Skip()) as P:
            tri1, t_c = P.sb([128, 128], BF16, "tri1")
            tri2, _ = P.sb([128, 128], BF16, "tri2")
            mask, _ = P.sb([128, 4, 2, 512], BF16, "mask")
            P.dma("pool", tri1[:], c_tri1, writes=[t_c])
            P.dma("pool", tri2[:], c_tri2, writes=[t_c])
            for j in range(4):
                for s_ in range(2):
                    P.dma("pool", mask[:, j, s_, :], c_mask[j], writes=[t_c])
            NB = 3
            Eb = [P.sb([128, 2, 512], F32, "E") for _ in range(NB)]
            Lb = [P.sb([128, 2, 512], BF16, "L") for _ in range(NB)]
            Xb = [P.sb([128, 2, 512], F32, "X") for _ in range(NB)]
            Wb = [P.sb([128, 2, 512], BF16, "W") for _ in range(NB)]
            Ob = [P.sb([64, 512], F32, "O") for _ in range(2)]
            Zp = [P.ps([128, 2, 512], F32, "Z") for _ in range(2)]
            Gp, t_g = P.ps([128, 2, 512], F32, "G")
            Op_ = [P.ps([64, 512], F32, "Oa") for _ in range(2)]
            class Seg:
                def __init__(self, hp, qc, n0):
                    self.hp, self.qc, self.n0 = hp, qc, n0
                    self.nb = 4 * qc + 4
                    self.q0 = qc * 512

                def diag(self, i):
                    return (self.nb - 1 - i) >= 4 * self.qc

                def cs(self, i):
                    j = self.nb - 1 - i - 4 * self.qc
                    return slice(128 * j, 512) if j > 0 else slice(0, 512)

                def qk(self, i):
                    kb = self.nb - 1 - i
                    z, t_z = Zp[(self.n0 + i) % 2]
                    c = self.cs(i)
                    for s in range(2):
                        pb = s * 64
                        P.mm(z[:, s, c], kT[pb:pb + 64, self.hp, kb * 128:(kb + 1) * 128],
                             qT[pb:pb + 64, self.hp, self.q0 + c.start:self.q0 + 512], True, True, writes=[t_z])

                def front(self, i):
                    (z, t_z), (E, t_E) = Zp[(self.n0 + i) % 2], Eb[(self.n0 + i) % NB]
                    c = self.cs(i)
                    P.act(E[:, :, c], z[:, :, c], AF.Exp, reads=[t_z], writes=[t_E], scale=0.125)

                def midA(self, i):
                    k = (self.n0 + i) % NB
                    (E, t_E), (L, t_L) = Eb[k], Lb[k]
                    c = self.cs(i)
                    P.act(L[:, :, c], E[:, :, c], AF.Ln, reads=[t_E], writes=[t_L], bias=1.0)
                    if self.diag(i):
                        j = self.nb - 1 - i - 4 * self.qc
                        P.tt("dve", L[:, :, c], L[:, :, c], mask[:, j, :, c], ALU.mult, reads=[t_L, t_c], writes=[t_L])

                def midB(self, i):
                    L, t_L = Lb[(self.n0 + i) % NB]
                    c = self.cs(i)
                    for s in range(2):
                        P.mm(Gp[:, s, c], tri1[:], L[:, s, c], i == 0, False, reads=[t_L, t_c], writes=[t_g])

                def backA(self, i):
                    k = (self.n0 + i) % NB
                    (L, t_L), (X, t_X) = Lb[k], Xb[k]
                    c = self.cs(i)
                    P.act(X[:, :, c], Gp[:, :, c], AF.Exp, reads=[t_g], writes=[t_X])
                    if i + 1 < self.nb:
                        for s in range(2):
                            P.mm(Gp[:, s, c], tri2[:], L[:, s, c], False, False, reads=[t_L, t_c], writes=[t_g])

                def backB(self, i):
                    k = (self.n0 + i) % NB
                    (E, t_E), (X, t_X), (W, t_W) = Eb[k], Xb[k], Wb[k]
                    c = self.cs(i)
                    P.tt("dve", W[:, :, c], E[:, :, c], X[:, :, c], ALU.mult, reads=[t_E, t_X], writes=[t_W])
                    if self.diag(i):
                        j = self.nb - 1 - i - 4 * self.qc
                        P.tt("dve", W[:, :, c], W[:, :, c], mask[:, j, :, c], ALU.mult, reads=[t_W, t_c], writes=[t_W])

                def pv(self, i):
                    kb = self.nb - 1 - i
                    W, t_W = Wb[(self.n0 + i) % NB]
                    c = self.cs(i)
                    for s in range(2):
                        o, t_o = Op_[s]
                        h = self.hp * 2 + s
                        P.mm(o[0:64, c], vv[:, kb, h * 64:(h + 1) * 64], W[:, s, c], i == 0, i == self.nb - 1,
                             reads=[t_W], writes=[t_o])

                def tail(self):
                    last = self.nb - 1
                    self.backB(last)
                    self.pv(last)
                    for s in range(2):
                        o, t_o = Op_[s]
                        ob, t_ob = Ob[s]
                        h = self.hp * 2 + s
                        P.copy("dve", ob[:], o[0:64, :], reads=[t_o], writes=[t_ob])
                        P.dma("sp", sbT_d[h * 64:(h + 1) * 64, self.q0:self.q0 + 512], ob[:], reads=[t_ob])

            segs = []
            n0 = 0
            for hp in range(4):
                for qc in range(NCH):
                    segs.append(Seg(hp, qc, n0))
                    n0 += segs[-1].nb
            segs[0].qk(0)
            prev = None
            for k, F in enumerate(segs):
                for i in range(F.nb):
                    F.front(i)
                    if i + 1 < F.nb:
                        F.qk(i + 1)
                    elif k + 1 < len(segs):
                        segs[k + 1].qk(0)
                    dg = F.diag(i)
                    if dg:
                        F.midA(i)
                    if i > 0:
                        F.backA(i - 1)
                    elif prev is not None:
                        prev.backA(prev.nb - 1)
                    if not dg:
                        F.midA(i)
                    F.midB(i)
                    if i > 0:
                        F.backB(i - 1)
                        F.pv(i - 1)
                    elif prev is not None:
                        prev.tail()
                prev = F
            prev.backA(prev.nb - 1)
            prev.tail()

        qkv.close()

        with (Phase(nc, "p3", G) if stop >= 3 else Skip()) as P:
            wp, t_wp = P.sb([128, 4, 128], BF16, "wp")
            psc, t_psc = P.sb([128, 4], F32, "psc")
            rc, t_rc = P.sb([128, 16], F32, "rc")
            for g in range(4):
                P.dma("pool", wp[:, g, :], w_pool[g], writes=[t_wp])
            P.dma("sp", psc[:], pool_scale, writes=[t_psc])
            P.dma("sp", rc[:], c_rcnt, writes=[t_rc])
            ub = [P.sb([128, SEQ], F32, "u") for _ in range(4)]
            wa2 = [[P.sb([128, SEQ], F32, "wa") for _ in range(2)] for _ in range(2)]
            pl = [P.sb([128, SEQ], BF16, "pl") for _ in range(2)]
            yst = [P.sb([128, 512], F32, "y") for _ in range(2)]
            pp = [P.ps([128, 512], F32, "pp") for _ in range(2)]
            k = 0
            for g in range(4):
                W = 2 << g
                u, t_u = ub[g]
                P.dma("act", u[:], uT_d[g * 128:(g + 1) * 128, :], writes=[t_u])
                eng = "dve" if g % 2 == 0 else "pool"
                wa = wa2[g % 2]
                src, t_src = u, t_u
                sh = 1
                step = 0
                while sh < W:
                    dst, t_dst = wa[step % 2]
                    P.tt(eng, dst[:, sh:SEQ], src[:, sh:SEQ], src[:, 0:SEQ - sh], ALU.add, reads=[t_src], writes=[t_dst])
                    P.copy(eng, dst[:, 0:sh], src[:, 0:sh], reads=[t_src], writes=[t_dst])
                    src, t_src = dst, t_dst
                    sh *= 2
                    step += 1
                p_, t_p = pl[g % 2]
                P.stt(p_[:, W:SEQ], src[:, W:SEQ], 1.0 / W, u[:, W:SEQ], ALU.mult, ALU.subtract,
                      reads=[t_src, t_u], writes=[t_p])
                tmp, t_tmp = wa[step % 2]
                P.tt("dve", tmp[:, 0:W], src[:, 0:W], rc[:, 0:W], ALU.mult, reads=[t_src, t_rc], writes=[t_tmp])
                P.tt("dve", p_[:, 0:W], tmp[:, 0:W], u[:, 0:W], ALU.subtract, reads=[t_tmp, t_u], writes=[t_p])
                for c in range(NCH):
                    pm, t_pm = pp[k % 2]
                    y, t_y = yst[k % 2]
                    k += 1
                    P.mm(pm[:], wp[:, g, :], p_[:, c * 512:(c + 1) * 512], True, True, reads=[t_wp, t_p], writes=[t_pm])
                    P.ts("dve", y[:], pm[:], psc[:, g:g + 1], None, ALU.mult, reads=[t_pm, t_psc], writes=[t_y])
                    P.dma("sp", yT_d[g * 128:(g + 1) * 128, c * 512:(c + 1) * 512], y[:], reads=[t_y])

        with (Phase(nc, "p4", G) if stop >= 4 else Skip()) as P:
            P.eps_ap = eps_t[:]
            wos, t_wosl = P.sb([128, 4, D], BF16, "wos", ntok=4)
            wop, t_wopl = P.sb([128, 4, D], BF16, "wop", ntok=4)
            gs, t_gs = P.sb([128, 4], F32, "gs")
            gp, t_gp = P.sb([128, 4], F32, "gp")
            for h in range(4):
                P.dma("pool", wos[:, h, :], w_out[h * 128:(h + 1) * 128, :], writes=[t_wosl[h]])
            for g in range(4):
                P.dma("pool", wop[:, g, :], w_out[512 + g * 128:512 + (g + 1) * 128, :], writes=[t_wopl[g]])
            P.dma("sp", gs[:], g_sb, writes=[t_gs])
            P.dma("sp", gp[:], g_pool, writes=[t_gp])
            sbc = [P.sb([128, 4, 512], F32, "sbc") for _ in range(2)]
            yc = [P.sb([128, 4, 512], F32, "yc") for _ in range(2)]
            asb = [P.sb([128, 4, 512], BF16, "asb", ntok=4) for _ in range(2)]
            apl = [P.sb([128, 4, 512], BF16, "apl", ntok=4) for _ in range(2)]
            sqs = [P.sb([128, 4, 512], BF16, "sqs") for _ in range(2)]
            sqp = [P.sb([128, 4, 512], BF16, "sqp") for _ in range(2)]
            xt = [P.sb([128, D], F32, "x") for _ in range(2)]
            ot = [P.sb([128, D], F32, "o") for _ in range(2)]
            rs = [P.sb([128, 2], F32, "rs") for _ in range(2)]
            pss = [P.ps([128, 2], F32, "pss") for _ in range(2)]
            po = [P.ps([128, D], F32, "po") for _ in range(2)]

            def prep4_load(c):
                (sc, t_sc), (y_, t_y) = sbc[c % 2], yc[c % 2]
                P.dma("sp", sc[:], sbT_d[:, c * 512:(c + 1) * 512].rearrange("(h d) t -> d h t", d=128), writes=[t_sc])
                P.dma("sp", y_[:], yT_d[:, c * 512:(c + 1) * 512].rearrange("(g e) t -> e g t", e=128), writes=[t_y])

            def prep4(c):
                (sc, t_sc), (y_, t_y), (a_s, t_as), (a_p, t_ap) = sbc[c % 2], yc[c % 2], asb[c % 2], apl[c % 2]
                (q_s, t_qs), (q_p, t_qp) = sqs[c % 2], sqp[c % 2]
                P.act(q_s[:], sc[:], AF.Square, reads=[t_sc], writes=[t_qs])
                P.act(q_p[:], y_[:], AF.Square, reads=[t_y], writes=[t_qp])
                for h in range(4):
                    if h % 2:
                        P.ts("dve", a_s[:, h, :], sc[:, h, :], gs[:, h:h + 1], None, ALU.mult, reads=[t_sc, t_gs], writes=[t_as[h]])
                    else:
                        P.act(a_s[:, h, :], sc[:, h, :], AF.Copy, reads=[t_sc, t_gs], writes=[t_as[h]], scale=gs[:, h:h + 1])
                for g in range(4):
                    if g % 2:
                        P.ts("dve", a_p[:, g, :], y_[:, g, :], gp[:, g:g + 1], None, ALU.mult, reads=[t_y, t_gp], writes=[t_ap[g]])
                    else:
                        P.act(a_p[:, g, :], y_[:, g, :], AF.Copy, reads=[t_y, t_gp], writes=[t_ap[g]], scale=gp[:, g:g + 1])

            def tile4(c, t):
                (a_s, t_as), (a_p, t_ap) = asb[c % 2], apl[c % 2]
                (q_s, t_qs), (q_p, t_qp) = sqs[c % 2], sqp[c % 2]
                ti = c * 4 + t
                tok0 = ti * 128
                tsl = slice(t * 128, (t + 1) * 128)
                if t == 2 and c + 1 < NCH:
                    prep4(c + 1)
                xs_, t_x = xt[ti % 2]
                o_, t_o = ot[ti % 2]
                r_, t_r = rs[ti % 2]
                ps_, t_ps = pss[ti % 2]
                pz, t_pz = po[ti % 2]
                P.dma("sp", xs_[:], x[tok0:tok0 + 128, :], writes=[t_x])
                for h in range(4):
                    P.mm(ps_[:, 0:1], q_s[:, h, tsl], ones_b[:, 0:1], h == 0, h == 3, reads=[t_qs], writes=[t_ps])
                yield
                P.act(r_[:, 0:1], ps_[:, 0:1], AF.Ln, reads=[t_ps], writes=[t_r], scale=1.0 / 512, bias=eps_t[:])
                yield
                for g in range(4):
                    P.mm(ps_[:, 1:2], q_p[:, g, tsl], ones_b[:, 0:1], g == 0, g == 3, reads=[t_qp], writes=[t_ps])
                yield
                P.act(r_[:, 1:2], ps_[:, 1:2], AF.Ln, reads=[t_ps], writes=[t_r], scale=1.0 / 512, bias=eps_t[:])
                yield
                P.act(r_[:], r_[:], AF.Exp, reads=[t_r], writes=[t_r], scale=-0.5)
                for nh in range(2):
                    for h in range(4):
                        P.mm(pz[:, nh * 512:(nh + 1) * 512], a_s[:, h, tsl], wos[:, h, nh * 512:(nh + 1) * 512],
                             h == 0, h == 3, reads=[t_as[h], t_wosl[h]], writes=[t_pz])
                yield
                for nh in range(2):
                    hs = slice(nh * 512, (nh + 1) * 512)
                    P.stt(o_[:, hs], pz[:, hs], r_[:, 0:1], xs_[:, hs], ALU.mult, ALU.add, reads=[t_pz, t_r, t_x], writes=[t_o])
                yield
                for nh in range(2):
                    for g in range(4):
                        P.mm(pz[:, nh * 512:(nh + 1) * 512], a_p[:, g, tsl], wop[:, g, nh * 512:(nh + 1) * 512],
                             g == 0, g == 3, reads=[t_ap[g], t_wopl[g]], writes=[t_pz])
                yield
                for nh in range(2):
                    hs = slice(nh * 512, (nh + 1) * 512)
                    P.stt(o_[:, hs], pz[:, hs], r_[:, 1:2], o_[:, hs], ALU.mult, ALU.add, reads=[t_pz, t_r, t_o], writes=[t_o])
                yield
                P.dma("sp", x1_d[tok0:tok0 + 128, :], o_[:], reads=[t_o])
                yield

            prep4_load(0)
            prep4(0)
            for c in range(NCH):
                if c + 1 < NCH:
                    prep4_load(c + 1)
                run_interleaved((tile4(c, t) for t in range(4)), 2)

        with (Phase(nc, "p5a", G) if stop >= 5 else Skip()) as P:
            P.eps_ap = eps_t[:]
            junk, t_junk = P.sb([128, D], BF16, "junk")
            ptr = [P.ps([128, 8, 128], BF16, "ptr") for _ in range(2)]
            pmm = [P.ps([128, 512], F32, "pmm") for _ in range(3)]
            wkv, t_wkvl = P.sb([128, 8, 2 * D], BF16, "wkv", ntok=8)
            gkv, t_gkv = P.sb([128, D], F32, "gkv")
            mT, t_mT = P.sb([128, 8, NMEM], BF16, "mT")
            mt = [P.sb([128, D], F32, "m") for _ in range(2)]
            mb = [P.sb([128, D], BF16, "mb") for _ in range(2)]
            mss = [P.sb([128, 1], F32, "mss") for _ in range(2)]
            for kc in range(8):
                P.dma("pool", wkv[:, kc, :], w_mem_kv[kc * 128:(kc + 1) * 128, :], writes=[t_wkvl[kc]])
            P.dma("sp", gkv[:], g_mem_kv.partition_broadcast(128), writes=[t_gkv])
            for t in range(2):
                (m_, t_m), (b_, t_b), (s_, t_s) = mt[t], mb[t], mss[t]
                pt, t_pt = ptr[t]
                P.dma("sp", m_[:], mem[t * 128:(t + 1) * 128, :], writes=[t_m])
                P.act(junk[:], m_[:], AF.Square, reads=[t_m], writes=[t_junk, t_s], accum_out=s_[:])
                P.rstd(s_[:], s_[:], D, t_s, t_s)
                P.stt(b_[:], m_[:], s_[:, 0:1], gkv[:], ALU.mult, ALU.mult, reads=[t_m, t_s, t_gkv], writes=[t_b])
                for kc in range(8):
                    P.tr(pt[:, kc, :], b_[:, kc * 128:(kc + 1) * 128], ident_b[:], reads=[t_b], writes=[t_pt])
                P.copy("dve", mT[:, :, t * 128:(t + 1) * 128], pt[:], reads=[t_pt], writes=[t_mT])
            mi = 0
            for fc in range(8):
                pm, t_pm = pmm[mi % 3]
                mi += 1
                for kc in range(8):
                    P.mm(pm[:, 0:NMEM], wkv[:, kc, fc * 128:(fc + 1) * 128], mT[:, kc, :], kc == 0, kc == 7,
                         reads=[t_wkvl[kc], t_mT], writes=[t_pm])
                P.copy("act" if fc % 2 else "dve", KT[:, fc, :], pm[:, 0:NMEM], reads=[t_pm], writes=[])
            for t in range(2):
                for nh in range(2):
                    pm, t_pm = pmm[mi % 3]
                    mi += 1
                    for kc in range(8):
                        P.mm(pm[:], mT[:, kc, t * 128:(t + 1) * 128], wkv[:, kc, D + nh * 512:D + (nh + 1) * 512],
                             kc == 0, kc == 7, reads=[t_wkvl[kc], t_mT], writes=[t_pm])
                    P.copy("act" if nh else "dve", V[:, t, nh * 512:(nh + 1) * 512], pm[:], reads=[t_pm], writes=[])

        with (Phase(nc, "p5", G) if stop >= 5 else Skip()) as P:
            P.eps_ap = eps_t[:]
            wq, t_wql = P.sb([128, 8, D], BF16, "wq", ntok=8)
            wo, t_wol = P.sb([128, 8, D], BF16, "wo", ntok=8)
            gq, t_gq = P.sb([128, D], F32, "gq")
            for kc in range(8):
                P.dma("pool", wq[:, kc, :], w_mem_q[kc * 128:(kc + 1) * 128, :], writes=[t_wql[kc]])
            for kc in range(8):
                P.dma("pool", wo[:, kc, :], w_mem_o[kc * 128:(kc + 1) * 128, :], writes=[t_wol[kc]])
            P.dma("sp", gq[:], g_mem_q.partition_broadcast(128), writes=[t_gq])
            ptr = [P.ps([128, 8, 128], BF16, "ptr") for _ in range(2)]
            pmm = [P.ps([128, 512], F32, "pmm") for _ in range(3)]
            psc_ = [P.ps([128, NMEM], F32, "psc") for _ in range(3)]
            NU = 5
            junks = [P.sb([128, D], BF16, "junk") for _ in range(2)]
            xc = [P.sb([128, 4, D], F32, "x1c") for _ in range(3)]
            hq = [P.sb([128, D], BF16, "hq") for _ in range(8)]
            sq = [P.sb([128, 1], F32, "sq") for _ in range(4)]
            hqTb = [P.sb([128, 8, 512], BF16, "hqT") for _ in range(2)]
            qmb = [P.sb([128, 8, 512], BF16, "qm", ntok=8) for _ in range(2)]
            pf = [P.sb([128, NMEM], F32, "pf") for _ in range(NU)]
            pb = [P.sb([128, NMEM], BF16, "pb") for _ in range(NU)]
            sm = [P.sb([128, 4], F32, "sm") for _ in range(NU)]
            pT, t_pTl = P.sb([128, 2, 4, 512], BF16, "pT", ntok=4)
            oT, t_oTl = P.sb([128, 8, 512], BF16, "oT", ntok=8)
            x2t = [P.sb([128, D], F32, "x2") for _ in range(2)]
            st5 = {"mi": 0}

            def load5(c):
                xc_, t_xc = xc[c % 3]
                P.dma("sp", xc_[:], x1_d[c * 512:(c + 1) * 512, :].rearrange("(t p) d -> p t d", p=128), writes=[t_xc])

            def tileA(c, t):
                xc_, t_xc = xc[c % 3]
                ti = c * 4 + t
                (h_, t_h), (s_, t_s) = hq[ti % 8], sq[ti % 4]
                junk, t_junk = junks[ti % 2]
                P.act(junk[:], xc_[:, t, :], AF.Square, reads=[t_xc], writes=[t_junk, t_s], accum_out=s_[:])
                yield
                P.act(s_[:], s_[:], AF.Ln, reads=[t_s], writes=[t_s], scale=1.0 / D, bias=eps_t[:])
                yield
                P.act(s_[:], s_[:], AF.Exp, reads=[t_s], writes=[t_s], scale=-0.5)
                yield
                P.stt(h_[:], xc_[:, t, :], s_[:, 0:1], gq[:], ALU.mult, ALU.mult, reads=[t_xc, t_s, t_gq], writes=[t_h])
                yield

            def trA(c):
                hqT, t_hqT = hqTb[c % 2]
                for t in range(4):
                    ti = c * 4 + t
                    h_, t_h = hq[ti % 8]
                    pt, t_pt = ptr[ti % 2]
                    for kc in range(8):
                        P.tr(pt[:, kc, :], h_[:, kc * 128:(kc + 1) * 128], ident_b[:], reads=[t_h], writes=[t_pt])
                    P.copy("dve" if t % 2 else "act", hqT[:, :, t * 128:(t + 1) * 128], pt[:], reads=[t_pt], writes=[t_hqT])

            def unitC(c, t, hd):
                k = (c * 4 + t) * 4 + hd
                qm, t_qml = qmb[c % 2]
                tsl = slice(t * 128, (t + 1) * 128)
                pscr, t_pscr = psc_[k % 3]
                (p_f, t_pf), (p_b, t_pb), (m_, t_m) = pf[k % NU], pb[k % NU], sm[k % NU]
                pt, t_pt = ptr[k % 2]
                for dc in range(2):
                    P.mm(pscr[:, 0:NMEM], qm[:, hd * 2 + dc, tsl], KT[:, hd * 2 + dc, :], dc == 0, dc == 1,
                         reads=[t_qml[hd * 2 + dc]], writes=[t_pscr])
                P.S.op("dve", lambda e: e.reduce_max(out=m_[:, 0:1], in_=pscr[:, 0:NMEM], axis=mybir.AxisListType.X),
                       [t_pscr], [t_m])
                P.ts("dve", m_[:, 1:2], m_[:, 0:1], -1.0 / 16, None, ALU.mult, reads=[t_m], writes=[t_m])
                P.act(p_f[:], pscr[:, 0:NMEM], AF.Exp, reads=[t_pscr, t_m], writes=[t_pf, t_m], scale=1.0 / 16,
                      bias=m_[:, 1:2], accum_out=m_[:, 2:3])
                yield
                P.S.op("dve", lambda e: e.reciprocal(m_[:, 3:4], m_[:, 2:3]), [t_m], [t_m])
                yield
                P.ts("dve", p_b[:], p_f[:], m_[:, 3:4], None, ALU.mult, reads=[t_pf, t_m], writes=[t_pb])
                yield
                for mc in range(2):
                    P.tr(pt[:, mc, :], p_b[:, mc * 128:(mc + 1) * 128], ident_b[:], reads=[t_pb], writes=[t_pt])
                P.copy("act", pT[:, :, hd, tsl], pt[:, 0:2, :], reads=[t_pt], writes=[t_pTl[hd]])
                yield

            def projB(c, fc):
                hqT, t_hqT = hqTb[c % 2]
                qm, t_qml = qmb[c % 2]
                pm, t_pm = pmm[st5["mi"] % 3]
                st5["mi"] += 1
                for kc in range(8):
                    P.mm(pm[:], wq[:, kc, fc * 128:(fc + 1) * 128], hqT[:, kc, :], kc == 0, kc == 7,
                         reads=[t_wql[kc], t_hqT], writes=[t_pm])
                P.copy("act" if fc % 2 else "dve", qm[:, fc, :], pm[:], reads=[t_pm], writes=[t_qml[fc]])

            def unitsC(c):
                gens = [unitC(c, t, hd) for t in range(4) for hd in range(4)]
                active = []
                nxt = 0
                fc = 0
                rounds = 0
                while nxt < len(gens) or active:
                    while nxt < len(gens) and len(active) < 4:
                        active.append(gens[nxt])
                        nxt += 1
                    for g_ in list(active):
                        try:
                            next(g_)
                        except StopIteration:
                            active.remove(g_)
                    rounds += 1
                    if c + 1 < NCH and fc < 8 and rounds % 2 == 0:
                        projB(c + 1, fc)
                        fc += 1
                while c + 1 < NCH and fc < 8:
                    projB(c + 1, fc)
                    fc += 1

            load5(0)
            run_interleaved((tileA(0, t) for t in range(4)), 2)
            trA(0)
            for fc in range(8):
                projB(0, fc)
            if NCH > 1:
                load5(1)
                run_interleaved((tileA(1, t) for t in range(4)), 2)
                trA(1)
            for c in range(NCH):
                xc_, t_xc = xc[c % 3]
                unitsC(c)
                if c + 2 < NCH:
                    load5(c + 2)
                    run_interleaved((tileA(c + 2, t) for t in range(4)), 2)
                for fc in range(8):
                    hd = fc // 2
                    pm, t_pm = pmm[st5["mi"] % 3]
                    st5["mi"] += 1
                    for mc in range(2):
                        P.mm(pm[:], V[:, mc, fc * 128:(fc + 1) * 128], pT[:, mc, hd, :], mc == 0, mc == 1,
                             reads=[t_pTl[hd]], writes=[t_pm])
                    P.copy("act" if fc % 2 else "dve", oT[:, fc, :], pm[:], reads=[t_pm], writes=[t_oTl[fc]])
                for t in range(4):
                    tok0 = c * 512 + t * 128
                    tsl = slice(t * 128, (t + 1) * 128)
                    x2_, t_x2t = x2t[t % 2]
                    for nh in range(2):
                        pm, t_pm = pmm[st5["mi"] % 3]
                        st5["mi"] += 1
                        for fc in range(8):
                            P.mm(pm[:], oT[:, fc, tsl], wo[:, fc, nh * 512:(nh + 1) * 512], fc == 0, fc == 7,
                                 reads=[t_oTl[fc], t_wol[fc]], writes=[t_pm])
                        P.tt("dve", x2_[:, nh * 512:(nh + 1) * 512], pm[:], xc_[:, t, nh * 512:(nh + 1) * 512], ALU.add,
                             reads=[t_pm, t_xc], writes=[t_x2t])
                    P.dma("sp", x2_d[tok0:tok0 + 128, :], x2_[:], reads=[t_x2t])
                if c + 2 < NCH:
                    trA(c + 2)

        with (Phase(nc, "p6", G) if stop >= 6 else Skip()) as P:
            P.eps_ap = eps_t[:]
            wr, t_wr = P.sb([128, 8, NE], F32, "wr")
            br, t_br = P.sb([128, NE], F32, "br")
            gf, t_gf = P.sb([128, D], F32, "gf")
            eoff, t_eo = P.sb([128, NE], F32, "eoff")
            ltri, t_lt = P.sb([128, 128], BF16, "ltri")
            cum, t_cum = P.sb([128, NE], F32, "cum")
            cumb, t_cumb = P.sb([128, NE], BF16, "cumb")
            P.dma("sp", wr[:], w_router.rearrange("(kc p) e -> p kc e", p=128), writes=[t_wr])
            P.dma("sp", br[:], b_router.partition_broadcast(128), writes=[t_br])
            P.dma("sp", gf[:], g_ffn.partition_broadcast(128), writes=[t_gf])
            P.dma("sp", eoff[:], c_eoff, writes=[t_eo])
            P.dma("pool", ltri[:], c_ltri, writes=[t_lt])
            P.S.op("dve", lambda e: e.memset(cum[:], 0.0), [], [t_cum])
            P.S.op("dve", lambda e: e.memset(cumb[:], 0.0), [], [t_cumb])
            NB6 = 4
            junks = [P.sb([128, D], BF16, "junk") for _ in range(NB6)]
            xt = [P.sb([128, D], F32, "x") for _ in range(NB6)]
            hf = [P.sb([128, D], F32, "hf") for _ in range(NB6)]
            hbb = [P.sb([128, D], BF16, "hb") for _ in range(NB6)]
            hT3 = [P.sb([128, 8, 128], F32, "hT3") for _ in range(NB6)]
            sq = [P.sb([128, 1], F32, "sq") for _ in range(NB6)]
            lg = [P.sb([128, NE], F32, "lg") for _ in range(NB6)]
            t8 = [P.sb([128, 8], F32, "t8") for _ in range(NB6)]
            sel = [P.sb([128, NE], BF16, "sel") for _ in range(NB6)]
            self_ = [P.sb([128, NE], F32, "self") for _ in range(NB6)]
            posf = [P.sb([128, NE], F32, "posf") for _ in range(NB6)]
            sc32 = [P.sb([128, NE], F32, "sc32") for _ in range(NB6)]
            pk = [P.sb([128, 4], F32, "pk") for _ in range(NB6)]
            pki = [P.sb([128, 4], I32, "pki") for _ in range(NB6)]
            gt = [P.sb([128, 8], F32, "gt") for _ in range(NB6)]
            ptr = [P.ps([128, 8, 128], F32, "ptr") for _ in range(2)]
            plg = [P.ps([128, NE], F32, "plg") for _ in range(2)]
            prk = [P.ps([128, NE], F32, "prk") for _ in range(2)]

            def tile6(ti):
                tok0 = ti * 128
                b = ti % NB6
                (x_, t_x), (h_, t_h), (hb_, t_hb), (hT_, t_hT), (s_, t_s) = xt[b], hf[b], hbb[b], hT3[b], sq[b]
                (l_, t_l), (t8_, t_t8), (se_, t_se), (sf_, t_sf), (po_, t_po), (sc_, t_scr) = lg[b], t8[b], sel[b], self_[b], posf[b], sc32[b]
                (pk_, t_pk), (pki_, t_pki), (g_, t_g) = pk[b], pki[b], gt[b]
                (pt, t_pt), (pl_, t_pl), (pr_, t_pr) = ptr[ti % 2], plg[ti % 2], prk[ti % 2]
                junk, t_junk = junks[b]
                P.dma("act", x_[:], x2_d[tok0:tok0 + 128, :], writes=[t_x])
                yield
                P.act(junk[:], x_[:], AF.Square, reads=[t_x], writes=[t_junk, t_s], accum_out=s_[:])
                yield
                P.act(s_[:], s_[:], AF.Ln, reads=[t_s], writes=[t_s], scale=1.0 / D, bias=eps_t[:])
                yield
                P.act(s_[:], s_[:], AF.Exp, reads=[t_s], writes=[t_s], scale=-0.5)
                yield
                P.stt(h_[:], x_[:], s_[:, 0:1], gf[:], ALU.mult, ALU.mult, reads=[t_x, t_s, t_gf], writes=[t_h])
                yield
                P.copy("act", hb_[:], h_[:], reads=[t_h], writes=[t_hb])
                for kc in range(8):
                    P.tr(pt[:, kc, :], h_[:, kc * 128:(kc + 1) * 128], ident_f[:], reads=[t_h], writes=[t_pt])
                P.copy("dve", hT_[:, 0:4, :], pt[:, 0:4, :], reads=[t_pt], writes=[t_hT])
                P.copy("act", hT_[:, 4:8, :], pt[:, 4:8, :], reads=[t_pt], writes=[t_hT])
                yield
                for kc in range(8):
                    P.mm(pl_[:, 0:NE], hT_[:, kc, :], wr[:, kc, :], kc == 0, kc == 7, reads=[t_hT, t_wr], writes=[t_pl])
                P.tt("dve", l_[:], pl_[:, 0:NE], br[:], ALU.add, reads=[t_pl, t_br], writes=[t_l])
                yield
                P.S.op("dve", lambda e, t8_=t8_, l_=l_: e.max(t8_[:], l_[:]), [t_l], [t_t8])
                yield
                P.ts("dve", sf_[:], l_[:], t8_[:, 3:4], None, ALU.is_ge, reads=[t_l, t_t8], writes=[t_sf])
                P.ts("pool", g_[:, 4:5], t8_[:, 0:1], -1.0, None, ALU.mult, reads=[t_t8], writes=[t_g])
                yield
                P.copy("dve", se_[:], sf_[:], reads=[t_sf], writes=[t_se])
                P.act(g_[:, 0:4], t8_[:, 0:4], AF.Exp, reads=[t_t8, t_g], writes=[t_g], bias=g_[:, 4:5], accum_out=g_[:, 5:6])
                yield
                P.mm(pr_[:, 0:NE], ltri[:], se_[:], True, False, reads=[t_lt, t_se], writes=[t_pr])
                P.mm(pr_[:, 0:NE], ones_b[:], cumb[:], False, True, reads=[t_cumb], writes=[t_pr])
                P.tt("dve", cum[:], cum[:], sf_[:], ALU.add, reads=[t_cum, t_sf], writes=[t_cum])
                P.copy("dve", cumb[:], cum[:], reads=[t_cum], writes=[t_cumb])
                P.ts("dve", sc_[:], pr_[:, 0:NE], float(CAP), 1.0e6, ALU.is_ge, ALU.mult, reads=[t_pr], writes=[t_scr])
                P.tt("dve", po_[:], pr_[:, 0:NE], eoff[:], ALU.add, reads=[t_pr, t_eo], writes=[t_po])
                yield
                P.S.op("dve", lambda e, g_=g_: e.reciprocal(g_[:, 6:7], g_[:, 5:6]), [t_g], [t_g])
                yield
                P.ts("dve", g_[:, 0:4], g_[:, 0:4], g_[:, 6:7], None, ALU.mult, reads=[t_g], writes=[t_g])
                yield
                P.dma("sp", gate_d[tok0:tok0 + 128, :], g_[:, 0:4], reads=[t_g])
                P.tt("dve", po_[:], po_[:], sc_[:], ALU.add, reads=[t_po, t_scr], writes=[t_po])
                yield
                P.ts("dve", po_[:], po_[:], float(TRASH), None, ALU.min, reads=[t_po], writes=[t_po])
                yield
                for k in range(4):
                    P.stt(sc_[:], l_[:], t8_[:, k:k + 1], po_[:], ALU.is_equal, ALU.mult, reads=[t_l, t_t8, t_po],
                          writes=[t_scr, t_pk], accum_out=pk_[:, k:k + 1])
                    yield
                P.copy("dve", pki_[:], pk_[:], reads=[t_pk], writes=[t_pki])
                yield
                P.dma("sp", pos_d[tok0:tok0 + 128, :], pki_[:], reads=[t_pki])
                for k in range(4):
                    P.S.dma("pool", lambda e, pki_=pki_, hb_=hb_, k=k: e.indirect_dma_start(
                        out=xs_d, out_offset=bass.IndirectOffsetOnAxis(ap=pki_[:, k:k + 1], axis=0),
                        in_=hb_[:], in_offset=None), [t_pki, t_hb], [])
                yield

            run_interleaved((tile6(ti) for ti in range(NT)), 3)

        with (Phase(nc, "p7", G) if stop >= 7 else Skip()) as P:
            w1b = [P.sb([128, 8, 2 * D], BF16, "w1b", ntok=8) for _ in range(2)]
            w2b = [P.sb([128, 8, D], BF16, "w2b", ntok=8) for _ in range(2)]
            b1t = [P.sb([128, 16], F32, "b1t") for _ in range(2)]
            b2t = [P.sb([128, D], F32, "b2t") for _ in range(2)]
            xr = [P.sb([128, CT, D], BF16, "xr") for _ in range(2)]
            xTb = [P.sb([128, 8, CAP], BF16, "xT") for _ in range(2)]
            NBUF = 3
            gtb = [P.sb([128, NH], F32, "gtb") for _ in range(NBUF)]
            sgb = [P.sb([128, NH], F32, "sgb") for _ in range(NBUF)]
            ubb = [P.sb([128, NH], F32, "ubb") for _ in range(NBUF)]
            aT = [P.sb([128, 8, NH], BF16, "aT") for _ in range(2)]
            yo = [P.sb([128, D], BF16, "yo") for _ in range(3)]
            ptr = [P.ps([128, 8, 128], BF16, "ptr") for _ in range(2)]
            pg = [P.ps([128, NH], F32, "pg") for _ in range(2)]
            pu = [P.ps([128, NH], F32, "pu") for _ in range(2)]
            py = [P.ps([128, 512], F32, "py") for _ in range(2)]
            st7 = {"gi": 0, "yi": 0, "pyi": 0}

            def load(ex):
                (w1_, t_w1), (w2_, t_w2), (b1_, t_b1), (b2_, t_b2), (xr_, t_xr) = \
                    w1b[ex % 2], w2b[ex % 2], b1t[ex % 2], b2t[ex % 2], xr[ex % 2]
                P.dma("sp", xr_[:], xs_d[ex * CAP:(ex + 1) * CAP, :].rearrange("(t p) d -> p t d", p=128), writes=[t_xr])
                for kc in range(8):
                    P.dma("pool", w1_[:, kc, :], w1[ex, kc * 128:(kc + 1) * 128, :], writes=[t_w1[kc]])
                for kc in range(8):
                    P.dma("pool", w2_[:, kc, :], w2[ex, kc * 128:(kc + 1) * 128, :], writes=[t_w2[kc]])
                P.dma("sp", b1_[:], b1[ex], writes=[t_b1])
                P.dma("sp", b2_[:], b2[ex].partition_broadcast(128), writes=[t_b2])

            def transposes(ex):
                xr_, t_xr = xr[ex % 2]
                xT, t_xT = xTb[ex % 2]
                for t in range(CT):
                    pt, t_pt = ptr[t % 2]
                    for kc in range(8):
                        P.tr(pt[:, kc, :], xr_[:, t, kc * 128:(kc + 1) * 128], ident_b[:], reads=[t_xr], writes=[t_pt])
                    P.copy("act" if t % 2 else "dve", xT[:, :, t * 128:(t + 1) * 128], pt[:], reads=[t_pt], writes=[t_xT])

            def pair_front(ex, sh, j):
                (w1_, t_w1), (b1_, t_b1) = w1b[ex % 2], b1t[ex % 2]
                xT, t_xT = xTb[ex % 2]
                a_, t_a = aT[(ex * NHALF + sh) % 2]
                ssl = slice(sh * NH, (sh + 1) * NH)
                gi = st7["gi"]
                st7["gi"] += 1
                (pg_, t_pg), (pu_, t_pu) = pg[gi % 2], pu[gi % 2]
                (g_, t_g), (s_, t_s), (u_, t_u) = gtb[gi % NBUF], sgb[gi % NBUF], ubb[gi % NBUF]
                for kc in range(8):
                    P.mm(pg_[:, 0:NH], w1_[:, kc, j * 128:(j + 1) * 128], xT[:, kc, ssl], kc == 0, kc == 7,
                         reads=[t_w1[kc], t_xT], writes=[t_pg])
                for kc in range(8):
                    P.mm(pu_[:, 0:NH], w1_[:, kc, D + j * 128:D + (j + 1) * 128], xT[:, kc, ssl], kc == 0, kc == 7,
                         reads=[t_w1[kc], t_xT], writes=[t_pu])
                P.ts("dve", g_[:], pg_[:, 0:NH], b1_[:, j:j + 1], 7.0, ALU.add, ALU.min, reads=[t_pg, t_b1], writes=[t_g])
                P.act(u_[:], pu_[:, 0:NH], AF.Identity, reads=[t_pu, t_b1], writes=[t_u], bias=b1_[:, 8 + j:9 + j])
                P.act(s_[:], g_[:], AF.Sigmoid, reads=[t_g], writes=[t_s], scale=1.702)
                P.ts("pool", u_[:], u_[:], 7.0, -7.0, ALU.min, ALU.max, reads=[t_u], writes=[t_u])
                P.tt("pool", s_[:], g_[:], s_[:], ALU.mult, reads=[t_g, t_s], writes=[t_s])

                def back():
                    P.stt(a_[:, j, :], u_[:], 1.0, s_[:], ALU.add, ALU.mult, reads=[t_u, t_s], writes=[t_a])
                return back

            def ymm(ex, sh):
                (w2_, t_w2), (b2_, t_b2) = w2b[ex % 2], b2t[ex % 2]
                a_, t_a = aT[(ex * NHALF + sh) % 2]
                for t in range(NH // 128):
                    row0 = ex * CAP + sh * NH + t * 128
                    y_, t_y = yo[st7["yi"] % 3]
                    st7["yi"] += 1
                    for nh in range(2):
                        py_, t_py = py[st7["pyi"] % 2]
                        st7["pyi"] += 1
                        for j in range(8):
                            P.mm(py_[:], a_[:, j, t * 128:(t + 1) * 128], w2_[:, j, nh * 512:(nh + 1) * 512], j == 0, j == 7,
                                 reads=[t_a, t_w2[j]], writes=[t_py])
                        P.tt("dve", y_[:, nh * 512:(nh + 1) * 512], py_[:], b2_[:, nh * 512:(nh + 1) * 512], ALU.add,
                             reads=[t_py, t_b2], writes=[t_y])
                    P.dma("sp", ys_d[row0:row0 + 128, :], y_[:], reads=[t_y])

            load(0)
            transposes(0)
            prev_unit = None
            for ex in range(NE):
                for sh in range(NHALF):
                    pend = None
                    for j in range(8):
                        back = pair_front(ex, sh, j)
                        if pend is not None:
                            pend()
                        pend = back
                        if j == 1 and prev_unit is not None:
                            ymm(*prev_unit)
                            prev_unit = None
                        if j == 1 and sh == 0 and ex + 1 < NE:
                            load(ex + 1)
                        if j == 4 and sh == NHALF - 1 and ex + 1 < NE:
                            transposes(ex + 1)
                    pend()
                    if prev_unit is not None:
                        ymm(*prev_unit)
                    prev_unit = (ex, sh)
            ymm(*prev_unit)

        with (Phase(nc, "p8", G) if stop >= 8 else Skip()) as P:
            P.eps_ap = eps_t[:]
            gfin, t_gfin = P.sb([128, D], F32, "gfin")
            P.dma("sp", gfin[:], g_final.partition_broadcast(128), writes=[t_gfin])
            NB8 = 7
            junks = [P.sb([128, D], BF16, "junk") for _ in range(NB8)]
            xt = [P.sb([128, D], F32, "x") for _ in range(NB8)]
            yk = [[P.sb([128, D], BF16, "yk") for _ in range(4)] for _ in range(NB8)]
            pki = [P.sb([128, 4], I32, "pki") for _ in range(NB8)]
            gt = [P.sb([128, 4], F32, "gt") for _ in range(NB8)]
            sq = [P.sb([128, 1], F32, "sq") for _ in range(NB8)]
            ot = [P.sb([128, D], F32, "o") for _ in range(NB8)]

            def tile8(ti):
                tok0 = ti * 128
                b = ti % NB8
                (x_, t_x), (pki_, t_pki), (g_, t_g), (s_, t_s), (o_, t_o) = xt[b], pki[b], gt[b], sq[b], ot[b]
                junk, t_junk = junks[b]
                P.dma("act", pki_[:], pos_d[tok0:tok0 + 128, :], writes=[t_pki])
                P.dma("act", x_[:], x2_d[tok0:tok0 + 128, :], writes=[t_x])
                P.dma("act", g_[:], gate_d[tok0:tok0 + 128, :], writes=[t_g])
                yield
                for k in range(4):
                    y_, t_y = yk[b][k]
                    P.S.dma("pool", lambda e, pki_=pki_, y_=y_, k=k: e.indirect_dma_start(
                        out=y_[:], out_offset=None, in_=ys_d,
                        in_offset=bass.IndirectOffsetOnAxis(ap=pki_[:, k:k + 1], axis=0)), [t_pki], [t_y])
                yield
                for k in range(4):
                    y_, t_y = yk[b][k]
                    P.stt(x_[:], y_[:], g_[:, k:k + 1], x_[:], ALU.mult, ALU.add, reads=[t_y, t_g, t_x], writes=[t_x])
                    yield
                P.act(junk[:], x_[:], AF.Square, reads=[t_x], writes=[t_junk, t_s], accum_out=s_[:])
                yield
                P.act(s_[:], s_[:], AF.Ln, reads=[t_s], writes=[t_s], scale=1.0 / D, bias=eps_t[:])
                yield
                P.act(s_[:], s_[:], AF.Exp, reads=[t_s], writes=[t_s], scale=-0.5)
                yield
                P.stt(o_[:], x_[:], s_[:, 0:1], gfin[:], ALU.mult, ALU.mult, reads=[t_x, t_s, t_gfin], writes=[t_o])
                yield
                P.dma("sp", out[tok0:tok0 + 128, :], o_[:], reads=[t_o])
                yield

            run_interleaved((tile8(ti) for ti in range(NT)), 6)
    return nc


def make_consts(CAP):
    j = np.arange(128)[:, None]
    s = np.arange(128)[None, :]
    c = {}
    c["c_ident"] = np.eye(128, dtype=np.float32)
    c["c_tri1"] = np.where(j >= s, -1.0, 0.0).astype(np.float32)
    c["c_tri2"] = np.where(j < s, -1.0, 0.0).astype(np.float32)
    c["c_ltri"] = np.where(j < s, 1.0, 0.0).astype(np.float32)
    c["c_ones"] = np.ones((128, 128), np.float32)
    m = np.zeros((4, 128, 512), np.float32)
    for jj in range(4):
        for cb in range(4):
            if cb > jj:
                m[jj, :, cb * 128:(cb + 1) * 128] = 1.0
            elif cb == jj:
                m[jj, :, cb * 128:(cb + 1) * 128] = (j < s).astype(np.float32)
    c["c_mask"] = m
    c["c_eoff"] = np.broadcast_to((np.arange(NE) * CAP).astype(np.float32)[None, :], (128, NE)).copy()
    c["c_rcnt"] = np.broadcast_to((1.0 / (np.arange(16) + 1)).astype(np.float32)[None, :], (128, 16)).copy()
    return c


def make_in_maps(inputs, n_cores, SEQ, CAP):
    f = lambda a: np.ascontiguousarray(np.asarray(a, dtype=np.float32))
    I = {k: np.asarray(v) for k, v in inputs.items()}
    shared = dict(make_consts(CAP))
    shared["g_mix"] = f(I["g_mix"][0][None, :])
    shared["w_in"] = f(I["w_in"][0])
    shared["g_sb"] = f(I["g_sb_out"][0].reshape(4, 128).T)
    shared["g_pool"] = f(I["g_pool_out"][0].reshape(4, 128).T)
    shared["w_pool"] = f(I["w_pool"][0])
    shared["pool_scale"] = f(I["pool_scale"][0].reshape(4, 128).T)
    shared["w_out"] = f(I["w_out"][0])
    shared["g_mem_q"] = f(I["g_mem_q"][0][None, :])
    shared["g_mem_kv"] = f(I["g_mem_kv"][0][None, :])
    shared["w_mem_q"] = f(I["w_mem_q"][0])
    shared["w_mem_kv"] = f(I["w_mem_kv"][0])
    shared["w_mem_o"] = f(I["w_mem_o"][0])
    shared["g_ffn"] = f(I["g_ffn"][0][None, :])
    shared["w_router"] = f(I["w_router"][0])
    shared["b_router"] = f(I["b_router"][0][None, :])
    shared["w1"] = f(I["w_expert_in"][0])
    shared["b1"] = f(I["b_expert_in"][0].reshape(NE, 16, 128).transpose(0, 2, 1))
    shared["w2"] = f(I["w_expert_out"][0])
    shared["b2"] = f(I["b_expert_out"][0][:, None, :])
    shared["g_final"] = f(I["g_final"][None, :])
    maps = []
    for c in range(n_cores):
        m = dict(shared)
        m["x"] = f(I["x"][c, :SEQ])
        m["mem"] = f(I["mem"][c])
        maps.append(m)
    return maps


def kernel(**inputs):
    SEQ, CAP = 4096, 768
    n = 8
    nc = build(SEQ, CAP)
    maps = make_in_maps(inputs, n, SEQ, CAP)
    res = run_bass_kernel_spmd(nc, maps, core_ids=list(range(n)))
    return np.stack([np.asarray(r["out"], dtype=np.float32) for r in res.results], axis=0)
```
